# Optimizing a Trainium2 kernel written in Bass

```python
import math
import jax, jax.numpy as jnp
from jax import lax
import numpy as np

D_MODEL = 2048
BATCH = 4
SEQ = 2048
DEPTH = 1
DEC_BATCH = 128
DEC_SEQ = 8
PAST_LEN = 16384
PAGE_SIZE = 128

HEAD_DIM = 64
N_HEADS = D_MODEL // HEAD_DIM
N_KV_HEADS = N_HEADS // 8
GROUP = N_HEADS // N_KV_HEADS
WINDOW = 128
ATTN_WIDTH = N_HEADS * HEAD_DIM
KV_WIDTH = N_KV_HEADS * HEAD_DIM
CONV_CH = D_MODEL
CONV_W = 31
N_EXPERTS = 32
TOP_K = 4
D_FF = D_MODEL
SWIGLU_ALPHA = 1.702
SWIGLU_LIMIT = 7.0
EPS = 1e-5
IN_SPLITS = (CONV_CH, 2 * CONV_CH, 2 * CONV_CH + ATTN_WIDTH,
             2 * CONV_CH + ATTN_WIDTH + KV_WIDTH, 2 * CONV_CH + ATTN_WIDTH + 2 * KV_WIDTH,
             2 * CONV_CH + ATTN_WIDTH + 2 * KV_WIDTH + D_MODEL)
IN_WIDTH = 2 * CONV_CH + ATTN_WIDTH + 2 * KV_WIDTH + 2 * D_MODEL

kernel_name = "hybrid_conv_swa_sink_moe_adaln_step"


def rmsnorm(x, g):
    xf = x.astype(jnp.float32)
    y = xf * lax.rsqrt(jnp.mean(xf * xf, axis=-1, keepdims=True) + EPS)
    return (y * g.astype(jnp.float32)).astype(x.dtype)


def sink_softmax(scores, sinks, mask):
    sink = sinks.astype(jnp.float32).reshape(N_KV_HEADS, GROUP, 1, 1)
    s = jnp.where(mask, scores, -jnp.inf)
    m = jnp.maximum(jnp.max(s, axis=-1, keepdims=True), sink)
    p = jnp.exp(s - m)
    return p / (jnp.sum(p, axis=-1, keepdims=True) + jnp.exp(sink - m))


def banded_window_attention(q, k, v, sinks):
    b, s = q.shape[:2]
    nb = s // WINDOW
    qb = q.reshape(b, nb, WINDOW, N_KV_HEADS, GROUP, HEAD_DIM)
    kb = k.reshape(b, nb, WINDOW, N_KV_HEADS, HEAD_DIM)
    vb = v.reshape(b, nb, WINDOW, N_KV_HEADS, HEAD_DIM)
    kk = jnp.concatenate([jnp.concatenate([jnp.zeros_like(kb[:, :1]), kb[:, :-1]], axis=1), kb], axis=2)
    vv = jnp.concatenate([jnp.concatenate([jnp.zeros_like(vb[:, :1]), vb[:, :-1]], axis=1), vb], axis=2)
    scores = jnp.einsum('bnqkgd,bnskd->bnkgqs', qb, kk,
                        preferred_element_type=jnp.float32) * (HEAD_DIM ** -0.5)
    i = jnp.arange(WINDOW)[:, None]
    j = jnp.arange(2 * WINDOW)[None, :]
    diff = i + WINDOW - j
    band = (diff >= 0) & (diff < WINDOW)
    blk = jnp.arange(nb)[:, None, None]
    mask = band[None] & ((blk > 0) | (j >= WINDOW)[None])
    p = sink_softmax(scores, sinks, mask[None, :, None, None])
    out = jnp.einsum('bnkgqs,bnskd->bnqkgd', p.astype(v.dtype), vv)
    return out.reshape(b, s, ATTN_WIDTH)


def window_cache_attention(q, k_new, v_new, k_buf, v_buf, sinks):
    b, L = q.shape[:2]
    nbuf = k_buf.shape[1]
    kk = jnp.concatenate([k_buf.astype(k_new.dtype), k_new], axis=1)
    vv = jnp.concatenate([v_buf.astype(v_new.dtype), v_new], axis=1)
    qpos = PAST_LEN + jnp.arange(L)
    kpos = PAST_LEN - nbuf + jnp.arange(nbuf + L)
    diff = qpos[:, None] - kpos[None, :]
    mask = (diff >= 0) & (diff < WINDOW)
    qg = q.reshape(b, L, N_KV_HEADS, GROUP, HEAD_DIM)
    scores = jnp.einsum('bqkgd,bskd->bkgqs', qg, kk,
                        preferred_element_type=jnp.float32) * (HEAD_DIM ** -0.5)
    p = sink_softmax(scores, sinks, mask)
    out = jnp.einsum('bkgqs,bskd->bqkgd', p.astype(vv.dtype), vv).reshape(b, L, ATTN_WIDTH)
    return out, kk[:, -nbuf:], vv[:, -nbuf:]


def conformer_conv(u_a, u_b, buf, dw_w, dw_b, ln_g, ln_b, w_co, b_co):
    u = u_a * jax.nn.sigmoid(u_b)
    up = jnp.concatenate([buf.astype(u.dtype), u], axis=1)
    z = lax.conv_general_dilated(up, dw_w[:, None, :].astype(u.dtype), window_strides=(1,),
                                 padding='VALID', dimension_numbers=('NWC', 'WIO', 'NWC'),
                                 feature_group_count=CONV_CH) + dw_b
    zf = z.astype(jnp.float32)
    mu = jnp.mean(zf, axis=-1, keepdims=True)
    var = jnp.var(zf, axis=-1, keepdims=True)
    zn = (zf - mu) * lax.rsqrt(var + EPS) * ln_g.astype(jnp.float32) + ln_b.astype(jnp.float32)
    zs = jax.nn.silu(zn).astype(u.dtype)
    return zs @ w_co + b_co, up[:, -(CONV_W - 1):]


def clamped_swiglu(a):
    x_glu = jnp.minimum(a[..., ::2], SWIGLU_LIMIT)
    x_lin = jnp.clip(a[..., 1::2], -SWIGLU_LIMIT, SWIGLU_LIMIT)
    return x_glu * jax.nn.sigmoid(SWIGLU_ALPHA * x_glu) * (x_lin + 1)


def moe(h, w_router, b_router, w_mlp1, b_mlp1, w_mlp2, b_mlp2):
    logits = (h @ w_router + b_router).astype(jnp.float32)
    vals, idx = lax.top_k(logits, TOP_K)
    gates = jax.nn.softmax(vals, axis=-1)
    comb = jnp.einsum('tk,tke->te', gates,
                      jax.nn.one_hot(idx, N_EXPERTS, dtype=jnp.float32)).astype(h.dtype)
    out = jnp.zeros_like(h)
    for e in range(N_EXPERTS):
        act = clamped_swiglu(h @ w_mlp1[e] + b_mlp1[e])
        out = out + comb[:, e:e + 1] * (act @ w_mlp2[e] + b_mlp2[e])
    return out


def setup_inputs(seed: int = 0) -> dict:
    key = jax.random.key(seed)
    ks = jax.random.split(key, 32)
    f32 = jnp.float32
    nrm = lambda k, shape, s: jax.random.normal(k, shape, f32) * s
    w_buf = min(WINDOW, PAST_LEN)
    return {
        "x_prompt": nrm(ks[0], (BATCH, SEQ, D_MODEL), 1.0),
        "x_sample": nrm(ks[1], (DEC_BATCH, DEC_SEQ, D_MODEL), 1.0),
        "c_prompt": nrm(ks[2], (BATCH, D_MODEL), 1.0),
        "c_sample": nrm(ks[3], (DEC_BATCH, D_MODEL), 1.0),
        "cache_k": nrm(ks[4], (DEC_BATCH, w_buf, N_KV_HEADS, HEAD_DIM), 1.0),
        "cache_v": nrm(ks[5], (DEC_BATCH, w_buf, N_KV_HEADS, HEAD_DIM), 1.0),
        "state_conv": nrm(ks[6], (DEC_BATCH, CONV_W - 1, CONV_CH), 1.0),
        "w_ada": nrm(ks[7], (D_MODEL, 6 * D_MODEL), 0.5 * D_MODEL ** -0.5),
        "b_ada": nrm(ks[8], (6 * D_MODEL,), 0.02),
        "norm1_g": 1.0 + nrm(ks[9], (D_MODEL,), 0.02),
        "w_in": nrm(ks[10], (D_MODEL, IN_WIDTH), D_MODEL ** -0.5),
        "b_in": nrm(ks[11], (IN_WIDTH,), 0.02),
        "conv_dw_w": nrm(ks[12], (CONV_W, CONV_CH), CONV_W ** -0.5),
        "conv_dw_b": nrm(ks[13], (CONV_CH,), 0.02),
        "conv_ln_g": 1.0 + nrm(ks[14], (CONV_CH,), 0.02),
        "conv_ln_b": nrm(ks[15], (CONV_CH,), 0.02),
        "w_conv_out": nrm(ks[16], (CONV_CH, D_MODEL), CONV_CH ** -0.5),
        "b_conv_out": nrm(ks[17], (D_MODEL,), 0.02),
        "sinks": nrm(ks[18], (N_HEADS,), 1.0),
        "w_attn_out": nrm(ks[19], (ATTN_WIDTH, D_MODEL), ATTN_WIDTH ** -0.5),
        "b_attn_out": nrm(ks[20], (D_MODEL,), 0.02),
        "w_out": nrm(ks[21], (D_MODEL, D_MODEL), D_MODEL ** -0.5),
        "norm2_g": 1.0 + nrm(ks[22], (D_MODEL,), 0.02),
        "w_router": nrm(ks[23], (D_MODEL, N_EXPERTS), D_MODEL ** -0.5),
        "b_router": nrm(ks[24], (N_EXPERTS,), 0.01),
        "w_mlp1": nrm(ks[25], (N_EXPERTS, D_MODEL, 2 * D_FF), D_MODEL ** -0.5),
        "b_mlp1": nrm(ks[26], (N_EXPERTS, 2 * D_FF), 0.02),
        "w_mlp2": nrm(ks[27], (N_EXPERTS, D_FF, D_MODEL), D_FF ** -0.5),
        "b_mlp2": nrm(ks[28], (N_EXPERTS, D_MODEL), 0.02),
        "norm_f_g": 1.0 + nrm(ks[29], (D_MODEL,), 0.02),
    }


def reference(x_prompt, x_sample, c_prompt, c_sample, cache_k, cache_v, state_conv,
              w_ada, b_ada, norm1_g, w_in, b_in, conv_dw_w, conv_dw_b, conv_ln_g, conv_ln_b,
              w_conv_out, b_conv_out, sinks, w_attn_out, b_attn_out, w_out, norm2_g,
              w_router, b_router, w_mlp1, b_mlp1, w_mlp2, b_mlp2, norm_f_g):
    def modulations(c):
        m = jax.nn.silu(c) @ w_ada + b_ada
        return [t[:, None, :] for t in jnp.split(m, 6, axis=-1)]

    def mixer_sublayer(x, mod, conv_buf, k_buf, v_buf, prompt):
        b, L = x.shape[:2]
        h = rmsnorm(x, norm1_g) * (1 + mod[1]) + mod[0]
        z = h @ w_in + b_in
        u_a, u_b, q, k, v, g_c, g_a = jnp.split(z, IN_SPLITS, axis=-1)
        q = q.reshape(b, L, N_HEADS, HEAD_DIM)
        k = k.reshape(b, L, N_KV_HEADS, HEAD_DIM)
        v = v.reshape(b, L, N_KV_HEADS, HEAD_DIM)
        conv_out, new_conv = conformer_conv(u_a, u_b, conv_buf, conv_dw_w, conv_dw_b,
                                            conv_ln_g, conv_ln_b, w_conv_out, b_conv_out)
        nbuf = k_buf.shape[1]
        if prompt:
            attn = banded_window_attention(q, k, v, sinks)
            new_k, new_v = k[:, -nbuf:], v[:, -nbuf:]
        else:
            attn, new_k, new_v = window_cache_attention(q, k, v, k_buf, v_buf, sinks)
        attn_out = attn @ w_attn_out + b_attn_out
        merged = jax.nn.sigmoid(g_c) * conv_out + jax.nn.sigmoid(g_a) * attn_out
        return x + mod[2] * (merged @ w_out), new_conv, new_k, new_v

    bp, sp = x_prompt.shape[:2]
    bs, ss = x_sample.shape[:2]
    mp = modulations(c_prompt)
    ms = modulations(c_sample)
    xp, xs = x_prompt, x_sample
    for _ in range(DEPTH):
        zero_conv = jnp.zeros((bp, CONV_W - 1, CONV_CH), xp.dtype)
        xp, conv_p, k_p, v_p = mixer_sublayer(xp, mp, zero_conv, cache_k[:bp], cache_v[:bp], True)
        xs, conv_s, k_s, v_s = mixer_sublayer(xs, ms, state_conv, cache_k, cache_v, False)
        h2p = rmsnorm(xp, norm2_g) * (1 + mp[4]) + mp[3]
        h2s = rmsnorm(xs, norm2_g) * (1 + ms[4]) + ms[3]
        tokens = jnp.concatenate([h2p.reshape(-1, D_MODEL), h2s.reshape(-1, D_MODEL)], axis=0)
        f = moe(tokens, w_router, b_router, w_mlp1, b_mlp1, w_mlp2, b_mlp2)
        xp = xp + mp[5] * f[:bp * sp].reshape(bp, sp, D_MODEL)
        xs = xs + ms[5] * f[bp * sp:].reshape(bs, ss, D_MODEL)
    y_prompt = rmsnorm(xp, norm_f_g)
    y_sample = rmsnorm(xs, norm_f_g)
    return (y_prompt, y_sample, k_p, v_p, conv_p, k_s, v_s, conv_s)
```

```python
import numpy as np
from contextlib import ExitStack
import concourse.bass as bass
import concourse.mybir as mybir
from concourse.bass_utils import run_bass_kernel_spmd

F32 = mybir.dt.float32
BF16 = mybir.dt.bfloat16
ALU = mybir.AluOpType
AF = mybir.ActivationFunctionType
AX = mybir.AxisListType

D = 2048
NK = 16
TH, TP, TS = 128, 1024, 128
NT = TH + TP + TS
NOWN = TP + TS
NB = 16
NE = 32
NR = 32
EPS = 1e-5
C_UA, C_UB, C_Q, C_K, C_V, C_GC, C_GA = 0, 2048, 4096, 6144, 6400, 6656, 8704
IN_W = 10752
GROUPS = [(0, 512), (512, 512), (1024, 256)]
OGROUPS = [(128, 512), (640, 512), (1152, 128)]


class Buf:
    __slots__ = ("name", "w", "r", "dsem", "dcnt")

    def __init__(self, name):
        self.name = name
        self.w = None
        self.r = {}
        self.dsem = None
        self.dcnt = 0


class K:
    def __init__(self, nc, st):
        self.nc = nc
        self.st = st
        self.eng = dict(pe=nc.tensor, act=nc.scalar, dve=nc.vector, pool=nc.gpsimd, sp=nc.sync)
        self.sem = {k: st.enter_context(nc.semaphore("s_" + k)) for k in self.eng}
        self.cnt = {k: 0 for k in self.eng}
        self.known = {k: {} for k in self.eng}
        self.semobj = {}
        for k, s in self.sem.items():
            self.semobj[s.num] = s
        self.nbuf = 0
        self.store_tokens = []
        self.alldma = {}

    def buf(self, name=None):
        self.nbuf += 1
        return Buf(f"{name or 'b'}_{self.nbuf}")

    def _waits(self, eng, reads, writes, skipw=False):
        waits = {}

        def need(tok):
            if tok is None:
                return
            s, v = tok
            if waits.get(s, 0) < v:
                waits[s] = v

        for b in reads:
            need(b.w)
        for b in writes:
            if not skipw:
                need(b.w)
            for s, v in b.r.items():
                need((s, v))
        kn = self.known[eng]
        e = self.eng[eng]
        for s, v in waits.items():
            if kn.get(s, 0) < v:
                kn[s] = v
                e.wait_ge(self.semobj[s], v)

    def op(self, eng, fn, reads=(), writes=()):
        self._waits(eng, reads, writes)
        ins = fn(self.eng[eng])
        sem = self.sem[eng]
        ins.then_inc(sem, 1)
        self.cnt[eng] += 1
        tok = (sem.num, self.cnt[eng])
        for b in reads:
            if b.r.get(tok[0], 0) < tok[1]:
                b.r[tok[0]] = tok[1]
        for b in writes:
            b.w = tok
            b.r = {}
        return tok

    def dma(self, eng, out, in_, owner, reads=(), writes=(), join=False, store=False):
        if owner.dsem is None:
            owner.dsem = self.st.enter_context(self.nc.semaphore("d_" + owner.name))
            self.semobj[owner.dsem.num] = owner.dsem
        self._waits(eng, reads, writes, skipw=join)
        self.eng[eng].dma_start(out=out, in_=in_).then_inc(owner.dsem, 16)
        owner.dcnt += 16
        tok = (owner.dsem.num, owner.dcnt)
        self.alldma[tok[0]] = tok[1]
        for b in reads:
            if b.r.get(tok[0], 0) < tok[1]:
                b.r[tok[0]] = tok[1]
        for b in writes:
            b.w = tok
            b.r = {}
        if store:
            self.store_tokens.append(tok)
        return tok

    def barrier(self):
        fin = dict(self.alldma)
        for k_ in self.eng:
            if self.cnt[k_]:
                fin[self.sem[k_].num] = self.cnt[k_]
        for eng, e in self.eng.items():
            kn = self.known[eng]
            for s_, v in fin.items():
                if kn.get(s_, 0) < v:
                    kn[s_] = v
                    e.wait_ge(self.semobj[s_], v)

    def finish(self):
        e = self.eng["sp"]
        fin = {}
        for s, v in self.store_tokens:
            fin[s] = max(fin.get(s, 0), v)
        for k in self.eng:
            if self.cnt[k]:
                fin[self.sem[k].num] = self.cnt[k]
        for s, v in fin.items():
            e.wait_ge(self.semobj[s], v)


def build(stage=9, n_exp=NE, dbg_names=()):
    nc = bass.Bass("TRN2", target_bir_lowering=False)

    def din(name, shape):
        return nc.dram_tensor(name, list(shape), F32, kind="ExternalInput").ap()

    def dout(name, shape):
        return nc.dram_tensor(name, list(shape), F32, kind="ExternalOutput").ap()

    xin = din("xin", [NT, D])
    c_own = din("c_own", [NR, D])
    cache_k = din("cache_k", [NB, 128, 256])
    cache_v = din("cache_v", [NB, 128, 256])
    state_conv = din("state_conv", [NB, 30, D])
    w_ada = din("w_ada", [D, 6 * D])
    b_ada = din("b_ada", [1, 6 * D])
    w_in = din("w_in", [D, IN_W])
    b_in = din("b_in", [1, IN_W])
    vec5 = din("vec5", [80, 128])
    conv_dw_w = din("conv_dw_w", [31, D])
    w_conv_out = din("w_conv_out", [D, D])
    b_conv_out = din("b_conv_out", [1, D])
    sinks_bc = din("sinks_bc", [128, 32])
    w_attn_out = din("w_attn_out", [D, D])
    b_attn_out = din("b_attn_out", [1, D])
    w_out = din("w_out", [D, D])
    w_router = din("w_router", [D, NE])
    b_router = din("b_router", [1, NE])
    normf_bc = din("normf_bc", [128, D])
    ident_in = din("ident", [128, 128])
    maskp0 = din("maskp0", [128, 256])
    maskp = din("maskp", [128, 256])
    maskc_in = din("maskc", [32, 128])
    maskw_in = din("maskw", [32, 248])
    hv_in = din("hv", [128, 1])
    sinks_s_in = din("sinks_s", [32, 8])
    sel_in = din("sel", [NR, 256])
    if stage >= 5:
        w_mlp1 = din("w_mlp1", [n_exp, D, 2 * D])
        b_mlp1 = din("b_mlp1", [n_exp, 1, 2 * D])
        w_mlp2 = din("w_mlp2", [n_exp, D, D])
        b_mlp2 = din("b_mlp2", [NE, D])

    y_out = dout("y", [NOWN, D])
    klast = dout("klast", [128, 256])
    vlast = dout("vlast", [128, 256])
    convlast = dout("convlast", [30, D])
    ks_out = dout("ks_out", [NB, 128, 256])
    vs_out = dout("vs_out", [NB, 128, 256])
    convs_out = dout("convs_out", [NB, 30, D])
    x1_d = nc.dram_tensor("x1_scratch", [NOWN, D], F32).ap()
    m5_d = nc.dram_tensor("m5_scratch", [2, 128, D], F32).ap()
    mtok_d = nc.dram_tensor("mtok_scratch", [NR, 2, D], F32).ap()

    with ExitStack() as st:
        E = st.enter_context
        k = K(nc, st)
        uid = [0]

        def sb(shape, dt, name=None, stack=st):
            uid[0] += 1
            return stack.enter_context(nc.sbuf_tensor(f"{name or 't'}{uid[0]}", list(shape), dt))

        dbgq = k.buf("dbgq")

        def dump(name, ap, bufs):
            if name not in dbg_names:
                return
            shp = list(ap.shape)
            o = dout("dbg_" + name, [shp[0], int(np.prod(shp[1:]))])
            src = ap
            if len(shp) == 3:
                o = o.rearrange("p (a b) -> p a b", a=shp[1])
            k.dma("pool", o, src, dbgq, reads=bufs, store=True)

        psum = E(nc.psum_tensor("psum", [128, 8, 512], F32))
        pbuf = [k.buf(f"ps{i}") for i in range(8)]
        prr = [0]

        def bank():
            i = prr[0] % 8
            prr[0] += 1
            return i

        def bank2():
            i = ((prr[0] + 1) // 2 * 2) % 8
            prr[0] = i + 2
            return i

        def mm(bi, out_ap, pairs):
            def fn(e):
                ins = None
                n = len(pairs)
                for j, (l, r) in enumerate(pairs):
                    ins = e.matmul(out_ap, l, r, start=(j == 0), stop=(j == n - 1))
                return ins
            return fn

        cst = k.buf("cst")
        idf = sb([128, 128], F32, "idf")
        idb = sb([128, 128], BF16, "idb")
        ones_b = sb([128, 512], BF16, "ones_b")
        ones_f = sb([1, 128], F32, "ones_f")
        k.dma("sp", idf[:], ident_in, cst, writes=[cst])
        c2 = k.buf("c2")
        k.op("dve", lambda e: e.tensor_copy(idb[:], idf[:]), reads=[cst], writes=[c2])
        k.op("dve", lambda e: e.memset(ones_b[:], 1.0), writes=[c2])
        k.op("dve", lambda e: e.memset(ones_f[:], 1.0), writes=[c2])

        NRING = 3
        WC = 256
        wring = [sb([128, NK + 1, WC], BF16, f"wr{i}") for i in range(NRING)]
        wbufs = [k.buf(f"wr{i}") for i in range(NRING)]
        wrr = [0]

        def wload(wmat, c0, brow=None, ncols=WC):
            i = wrr[0] % NRING
            wrr[0] += 1
            t, bf = wring[i], wbufs[i]
            src = wmat.rearrange("(k p) c -> p k c", p=128)[:, :, c0:c0 + ncols]
            if brow is not None:
                k.dma("pool", t[0:1, NK, 0:ncols], brow[0:1, c0:c0 + ncols], bf, writes=[bf])
            k.dma("pool", t[:, 0:NK, 0:ncols], src, bf, writes=[bf], join=(brow is not None))
            return t, bf

        hT = sb([128, NK, NT], BF16, "hT")
        hb = [k.buf(f"hT{i}") for i in range(10)]
        h2T = hT[:, :, 0:NOWN]
        h2b = [k.buf(f"h2T{i}") for i in range(9)]
        comb = sb([128, 9, NE], F32, "comb")
        combb = k.buf("comb")

        mix = ExitStack()
        gT = sb([128, 80], F32, "gT", mix)
        dwT = sb([128, NK, 31], F32, "dwT", mix)
        sinks = sb([128, 32], F32, "sinks", mix)
        hv = sb([128, 1], F32, "hv", mix)
        sel = sb([NR, 256], F32, "sel", mix)
        mk0 = sb([128, 256], F32, "mk0", mix)
        mk1 = sb([128, 256], F32, "mk1", mix)
        k.dma("sp", sinks[:], sinks_bc, cst, writes=[cst], join=True)
        k.dma("sp", hv[:], hv_in, cst, writes=[cst], join=True)
        k.dma("sp", sel[:], sel_in, cst, writes=[cst], join=True)
        k.dma("sp", mk0[:], maskp0, cst, writes=[cst], join=True)
        k.dma("sp", mk1[:], maskp, cst, writes=[cst], join=True)

        with ExitStack() as s0:
            v5 = sb([80, 128], F32, "v5", s0)
            dw = sb([31, D], F32, "dw", s0)
            ld = k.buf("ld0")
            k.dma("sp", v5[:], vec5, ld, writes=[ld])
            k.dma("sp", dw[:], conv_dw_w, ld, writes=[ld], join=True)
            b = bank()
            k.op("pe", lambda e: e.transpose(psum[:, b, 0:80], v5[:], idf[0:80, 0:80]), reads=[ld, cst], writes=[pbuf[b]])
            k.op("dve", lambda e: e.tensor_copy(gT[:], psum[:, b, 0:80]), reads=[pbuf[b]], writes=[c2])
            for c in range(NK):
                b = bank()
                k.op("pe", lambda e: e.transpose(psum[:, b, 0:31], dw[:, c * 128:(c + 1) * 128], idf[0:31, 0:31]),
                     reads=[ld, cst], writes=[pbuf[b]])
                k.op("dve", lambda e: e.tensor_copy(dwT[:, c, :], psum[:, b, 0:31]), reads=[pbuf[b]], writes=[c2])
            k.barrier()
        G1, DWB, LNG, LNB, G2 = 0, 16, 32, 48, 64

        NM = 17
        modT = sb([128, 4, NK, NM], F32, "modT", mix)
        S1T = sb([128, NK, NM], F32, "S1T", mix)
        S2T = sb([128, NK, NM], F32, "S2T", mix)
        modb = k.buf("modT")
        mtokb = k.buf("mtok")
        with ExitStack() as s0:
            cs = sb([NR, D], F32, "cs", s0)
            mtok = sb([NR, 2, D], F32, "mtok", s0)
            scT = sb([128, NK, NR], BF16, "scT", s0)
            csb = k.buf("cs")
            k.dma("sp", cs[:], c_own, csb, writes=[csb])
            k.op("act", lambda e: e.activation(cs[:], cs[:], AF.Silu), reads=[csb], writes=[csb])
            scb = k.buf("scT")
            for kk in range(NK):
                b = bank()
                k.op("pe", lambda e: e.transpose(psum[:, b, 0:NR], cs[:, kk * 128:(kk + 1) * 128], idf[0:NR, 0:NR]),
                     reads=[csb, cst], writes=[pbuf[b]])
                k.op("dve", lambda e: e.tensor_copy(scT[:, kk, :], psum[:, b, 0:NR]), reads=[pbuf[b]], writes=[scb])
            fm_slot = {0: 0, 1: 1, 3: 2, 4: 3}
            for mi in range(6):
                for cb in range(D // WC):
                    c0 = mi * D + cb * WC
                    t, bf = wload(w_ada, c0, b_ada)
                    if mi in fm_slot:
                        for j in range(WC // 128):
                            b = bank()
                            pairs = [(t[:, kk, j * 128:(j + 1) * 128], scT[:, kk, :]) for kk in range(NK)]
                            pairs.append((t[0:1, NK, j * 128:(j + 1) * 128], ones_b[0:1, 0:NR]))
                            k.op("pe", mm(b, psum[:, b, 0:NR], pairs), reads=[bf, scb, c2], writes=[pbuf[b]])
                            ch = cb * (WC // 128) + j
                            k.op("act", lambda e: e.copy(modT[:, fm_slot[mi], ch, :], psum[:, b, 0:NM]),
                                 reads=[pbuf[b]], writes=[modb])
                    else:
                        b = bank()
                        pairs = [(scT[:, kk, :], t[:, kk, :]) for kk in range(NK)]
                        pairs.append((ones_b[0:1, 0:NR], t[0:1, NK, :]))
                        k.op("pe", mm(b, psum[0:NR, b, 0:WC], pairs), reads=[bf, scb, c2], writes=[pbuf[b]])
                        k.op("act", lambda e: e.copy(mtok[:, 0 if mi == 2 else 1, cb * WC:(cb + 1) * WC], psum[0:NR, b, 0:WC]),
                             reads=[pbuf[b]], writes=[mtokb])
            mtokd_b = k.buf("mtokd")
            k.dma("sp", mtok_d, mtok[:], mtokb, reads=[mtokb], writes=[mtokd_b])
            for (S, slot, goff) in ((S1T, 1, G1), (S2T, 3, G2)):
                k.op("dve", lambda e: e.tensor_scalar(S[:], modT[:, slot, :, :], 1.0, None, ALU.add), reads=[modb], writes=[modb])
                k.op("dve", lambda e: e.tensor_tensor(S[:], S[:], gT[:, goff:goff + NK].unsqueeze(2).to_broadcast([128, NK, NM]), ALU.mult),
                     reads=[modb, c2], writes=[modb])
            k.barrier()
        B1T = modT[:, 0, :, :]
        B2T = modT[:, 2, :, :]

        def norm_alloc(stk):
            xts_ = [sb([128, D], F32, f"xt{i}", stk) for i in range(2)]
            xtb_ = [k.buf(f"xt{i}") for i in range(2)]
            return (xts_, xtb_, sb([128, D], BF16, "junk", stk), k.buf("junk"), sb([128, 8], F32, "stat", stk), k.buf("stat"),
                    sb([128, 512], F32, "mtmp", stk), k.buf("mtmp"))
        sB = ExitStack()
        xts, xtb, junk, junkb, stat, statb, mtmp, mtmpb = norm_alloc(sB)

        def rms_rstd(xt, xb):
            k.op("act", lambda e: e.activation(junk[:], xt[:], AF.Square, accum_out=stat[:, 0:1]), reads=[xb], writes=[junkb, statb])
            k.op("dve", lambda e: e.tensor_scalar(stat[:, 1:2], stat[:, 0:1], 1.0 / D, EPS, ALU.mult, ALU.add), reads=[statb], writes=[statb])
            k.op("act", lambda e: e.activation(stat[:, 3:4], stat[:, 1:2], AF.Sqrt), reads=[statb], writes=[statb])
            k.op("dve", lambda e: e.reciprocal(stat[:, 2:3], stat[:, 3:4]), reads=[statb], writes=[statb])

        def norm_mod_T(xt, xb, S, Bm, sample, dst_fn, dst_bufs, f32_dst=None):
            rms_rstd(xt, xb)
            k.op("dve", lambda e: e.tensor_scalar(xt[:], xt[:], stat[:, 2:3], 0.0, ALU.mult, ALU.add), reads=[xb, statb], writes=[xb])
            for q4 in range(4):
                b = bank()

                def tr(e):
                    ins = None
                    for j in range(4):
                        kk = q4 * 4 + j
                        ins = e.transpose(psum[:, b, j * 128:(j + 1) * 128], xt[:, kk * 128:(kk + 1) * 128], idf[:])
                    return ins
                k.op("pe", tr, reads=[xb, cst], writes=[pbuf[b]])
                pv = psum[:, b, :].rearrange("p (c t) -> p c t", c=4)
                tv = mtmp[:].rearrange("p (c t) -> p c t", c=4)
                if not sample:
                    sbc = S[:, q4 * 4:q4 * 4 + 4, 0:1].to_broadcast([128, 4, 128])
                    bbc = Bm[:, q4 * 4:q4 * 4 + 4, 0:1].to_broadcast([128, 4, 128])
                    pvv, tvv = pv, tv
                    dst = dst_fn(q4 * 4)
                else:
                    sbc = S[:, q4 * 4:q4 * 4 + 4, 1:17].unsqueeze(3).to_broadcast([128, 4, NB, 8])
                    bbc = Bm[:, q4 * 4:q4 * 4 + 4, 1:17].unsqueeze(3).to_broadcast([128, 4, NB, 8])
                    pvv = psum[:, b, :].rearrange("p (c b l) -> p c b l", c=4, b=NB)
                    tvv = mtmp[:].rearrange("p (c b l) -> p c b l", c=4, b=NB)
                    dst = dst_fn(q4 * 4).rearrange("p c (b l) -> p c b l", b=NB)
                k.op("dve", lambda e: e.tensor_tensor(tvv, pvv, sbc, ALU.mult), reads=[pbuf[b], modb], writes=[mtmpb])
                k.op("dve", lambda e: e.tensor_tensor(dst, tvv, bbc, ALU.add), reads=[mtmpb, modb], writes=dst_bufs)
                if f32_dst is not None:
                    fd, fb = f32_dst
                    fdv = fd[:, q4 * 4:q4 * 4 + 4, :]
                    if sample:
                        fdv = fdv.rearrange("p c (b l) -> p c b l", b=NB)
                    k.op("dve", lambda e: e.tensor_tensor(fdv, tvv, bbc, ALU.add), reads=[mtmpb, modb], writes=[fb])

        for i in range(10):
            xt, xb = xts[i % 2], xtb[i % 2]
            k.dma("sp", xt[:], xin[i * 128:(i + 1) * 128, :], xb, writes=[xb])
            norm_mod_T(xt, xb, S1T, B1T, i == 9, lambda kk0, i=i: hT[:, kk0:kk0 + 4, i * 128:(i + 1) * 128], [hb[i]])
        k.barrier()
        sB.close()

        def hgrp_bufs(t0, n):
            return [hb[i] for i in range(t0 // 128, (t0 + n + 127) // 128)]

        def proj_fm(t, bf, j, t0, n):
            b = bank()
            pairs = [(t[:, kk, j * 128:(j + 1) * 128], hT[:, kk, t0:t0 + n]) for kk in range(NK)]
            pairs.append((t[0:1, NK, j * 128:(j + 1) * 128], ones_b[0:1, 0:n]))
            k.op("pe", mm(b, psum[:, b, 0:n], pairs), reads=[bf, c2] + hgrp_bufs(t0, n), writes=[pbuf[b]])
            return b

        with ExitStack() as s1:
            kvo = sb([128, 4, 256], F32, "kvo", s1)
            kvob = k.buf("kvo")
            for wi, c0 in enumerate((C_K, C_V)):
                tkv, bkv = wload(w_in, c0, b_in)
                for ti, i in enumerate((8, 9)):
                    b = bank()
                    pairs = [(hT[:, kk, i * 128:(i + 1) * 128], tkv[:, kk, :]) for kk in range(NK)]
                    pairs.append((ones_b[0:1, 0:128], tkv[0:1, NK, :]))
                    k.op("pe", mm(b, psum[:, b, 0:256], pairs), reads=[bkv, c2, hb[i]], writes=[pbuf[b]])
                    k.op("act", lambda e: e.copy(kvo[:, wi * 2 + ti, :], psum[:, b, 0:256]), reads=[pbuf[b]], writes=[kvob])
            k.dma("sp", klast, kvo[:, 0, :], kvob, reads=[kvob], store=True)
            k.dma("sp", vlast, kvo[:, 2, :], kvob, reads=[kvob], store=True)
            for bi in range(NB):
                k.dma("sp", ks_out[bi, 120:128, :], kvo[bi * 8:(bi + 1) * 8, 1, :], kvob, reads=[kvob], store=True)
                k.dma("sp", vs_out[bi, 120:128, :], kvo[bi * 8:(bi + 1) * 8, 3, :], kvob, reads=[kvob], store=True)
            k.dma("sp", ks_out[:, 0:120, :], cache_k[:, 8:128, :], kvob, store=True)
            k.dma("sp", vs_out[:, 0:120, :], cache_v[:, 8:128, :], kvob, store=True)
            k.barrier()

        mergedT = sb([128, NK, NOWN], BF16, "mergedT", mix)
        mgb = [k.buf(f"mg{c}") for c in range(NK)]
        cv = ExitStack()
        zT = sb([128, NK, NOWN], BF16, "zT", cv)
        zb = [k.buf(f"z{c}") for c in range(NK)]
        with ExitStack() as s1:
            uT = sb([128, NT], F32, "uT", s1)
            ub = k.buf("uT")
            sg = sb([128, 512], F32, "sg", s1)
            sgb = k.buf("sg")
            uTb = sb([128, NT], BF16, "uTb", s1)
            uTbb = k.buf("uTb")
            dg = sb([128, 31, 128], BF16, "dg", s1)
            dgb = k.buf("dg")
            uS = sb([128, 2, 128], F32, "uS", s1)
            uSb = [k.buf("uS0"), k.buf("uS1")]
            uL = sb([30, 2, 128], F32, "uL", s1)
            uLb = [k.buf("uL0"), k.buf("uL1")]
            stt = sb([120, 4, 128], F32, "stt", s1)
            sttb = k.buf("stt")
            upS = sb([128, NB, 38], F32, "upS", s1)
            upb = k.buf("upS")
            accS = sb([128, NB, 8], F32, "accS", s1)
            accSb = k.buf("accS")
            for cb in range(D // WC):
                ta, bfa = wload(w_in, C_UA + cb * WC, b_in)
                tb_, bfb = wload(w_in, C_UB + cb * WC, b_in)
                for j in range(WC // 128):
                    ch = cb * (WC // 128) + j
                    k.dma("sp", stt[:], state_conv.rearrange("(q b) r d -> (b r) q d", q=4)[:, :, ch * 128:(ch + 1) * 128], sttb, writes=[sttb])
                    for (t0, n) in GROUPS:
                        ba = proj_fm(ta, bfa, j, t0, n)
                        bb = proj_fm(tb_, bfb, j, t0, n)
                        k.op("act", lambda e: e.activation(sg[:, 0:n], psum[:, bb, 0:n], AF.Sigmoid), reads=[pbuf[bb]], writes=[sgb])
                        k.op("dve", lambda e: e.tensor_tensor(uT[:, t0:t0 + n], psum[:, ba, 0:n], sg[:, 0:n], ALU.mult),
                             reads=[pbuf[ba], sgb], writes=[ub])
                    k.op("dve", lambda e: e.tensor_scalar(uT[:, 0:TH], uT[:, 0:TH], hv[:, 0:1], 0.0, ALU.mult, ALU.add), reads=[ub, cst], writes=[ub])
                    b = bank()

                    def tr2(e):
                        e.transpose(psum[:, b, 0:128], uT[:, TH + TP:NT], idf[:])
                        return e.transpose(psum[0:30, b, 128:256], uT[:, TH + TP - 30:TH + TP], idf[:])
                    k.op("pe", tr2, reads=[ub, cst], writes=[pbuf[b]])
                    r_ = ch % 2
                    k.op("act", lambda e: e.copy(uS[:, r_, :], psum[:, b, 0:128]), reads=[pbuf[b]], writes=[uSb[r_]])
                    k.op("act", lambda e: e.copy(uL[:, r_, :], psum[0:30, b, 128:256]), reads=[pbuf[b]], writes=[uLb[r_]])
                    k.dma("sp", convlast[:, ch * 128:(ch + 1) * 128], uL[:, r_, :], uLb[r_], reads=[uLb[r_]], store=True)
                    for bi in range(NB):
                        k.dma("sp", convs_out[bi, 22:30, ch * 128:(ch + 1) * 128], uS[bi * 8:(bi + 1) * 8, r_, :], uSb[r_], reads=[uSb[r_]], store=True)
                    b = bank()

                    def tr3(e):
                        ins = None
                        for q in range(4):
                            ins = e.transpose(psum[:, b, q * 120:(q + 1) * 120], stt[:, q, :], idf[0:120, 0:120])
                        return ins
                    k.op("pe", tr3, reads=[sttb, cst], writes=[pbuf[b]])
                    k.op("act", lambda e: e.copy(upS[:, :, 0:30], psum[:, b, 0:480].rearrange("p (b r) -> p b r", r=30)), reads=[pbuf[b]], writes=[upb])
                    k.op("act", lambda e: e.copy(upS[:, :, 30:38], uT[:, TH + TP:NT].rearrange("p (b l) -> p b l", l=8)), reads=[ub], writes=[upb])
                    o0 = TH - 30
                    k.op("act", lambda e: e.copy(uTb[:, 0:TH + TP], uT[:, 0:TH + TP]), reads=[ub], writes=[uTbb])
                    k.op("dve", lambda e: e.tensor_tensor(dg[:], idb[:].unsqueeze(1).to_broadcast([128, 31, 128]),
                                                           dwT[:, ch, :].unsqueeze(2).to_broadcast([128, 31, 128]), ALU.mult), reads=[c2], writes=[dgb])
                    for t0c in (0, 512):
                        b = bank()
                        pairs = [(dg[:, jj, :], uTb[:, o0 + jj + t0c:o0 + jj + t0c + 512]) for jj in range(31)]
                        k.op("pe", mm(b, psum[:, b, 0:512], pairs), reads=[dgb, uTbb], writes=[pbuf[b]])
                        k.op("act", lambda e: e.activation(zT[:, ch, t0c:t0c + 512], psum[:, b, 0:512], AF.Identity, bias=gT[:, DWB + ch:DWB + ch + 1]),
                             reads=[pbuf[b], c2], writes=[zb[ch]])
                    k.op("dve", lambda e: e.tensor_scalar(accS[:], upS[:, :, 0:8], dwT[:, ch, 0:1], gT[:, DWB + ch:DWB + ch + 1], ALU.mult, ALU.add),
                         reads=[upb, c2], writes=[accSb])
                    for jj in range(1, 31):
                        last = jj == 30
                        dst = zT[:, ch, TP:NOWN].rearrange("p (b l) -> p b l", l=8) if last else accS[:]
                        k.op("dve", lambda e: e.scalar_tensor_tensor(dst, upS[:, :, jj:jj + 8], dwT[:, ch, jj:jj + 1], accS[:], ALU.mult, ALU.add),
                             reads=[upb, accSb, c2], writes=[zb[ch]] if last else [accSb])
            k.dma("sp", convs_out[:, 0:22, :], state_conv[:, 8:30, :], uSb[0], store=True)
            k.barrier()
        dump("zT", zT[:], zb)

        with ExitStack() as s1:
            lnA = sb([128, NOWN], F32, "lnA", s1)
            lnB = sb([128, NOWN], F32, "lnB", s1)
            lnb_ = k.buf("ln")
            zsq = sb([128, 512], BF16, "zsq", s1)
            zsqb = k.buf("zsq")
            lt = sb([128, 512], F32, "lt", s1)
            ltb = k.buf("lt")
            for (t0, n) in [(0, 512), (512, 512), (1024, 128)]:
                b1 = bank()
                b2 = bank()
                k.op("pe", mm(b1, psum[:, b1, 0:n], [(ones_b[:, 0:128], zT[:, c, t0:t0 + n]) for c in range(NK)]), reads=zb + [c2], writes=[pbuf[b1]])
                for c in range(NK):
                    k.op("act", lambda e: e.activation(zsq[:, 0:n], zT[:, c, t0:t0 + n], AF.Square), reads=[zb[c]], writes=[zsqb])
                    k.op("pe", lambda e: e.matmul(psum[:, b2, 0:n], ones_b[:, 0:128], zsq[:, 0:n], start=(c == 0), stop=(c == NK - 1)),
                         reads=[zsqb, c2], writes=[pbuf[b2]])
                A = lnA[:, t0:t0 + n]
                Bv = lnB[:, t0:t0 + n]
                k.op("dve", lambda e: e.tensor_scalar(Bv, psum[:, b1, 0:n], 1.0 / D, None, ALU.mult), reads=[pbuf[b1]], writes=[lnb_])
                k.op("dve", lambda e: e.tensor_tensor(lt[:, 0:n], Bv, Bv, ALU.mult), reads=[lnb_], writes=[ltb])
                k.op("dve", lambda e: e.scalar_tensor_tensor(A, psum[:, b2, 0:n], 1.0 / D, lt[:, 0:n], ALU.mult, ALU.subtract), reads=[pbuf[b2], ltb], writes=[lnb_])
                k.op("dve", lambda e: e.tensor_scalar(A, A, EPS, None, ALU.add), reads=[lnb_], writes=[lnb_])
                k.op("act", lambda e: e.activation(A, A, AF.Sqrt), reads=[lnb_], writes=[lnb_])
                k.op("dve", lambda e: e.reciprocal(A, A), reads=[lnb_], writes=[lnb_])
                k.op("dve", lambda e: e.scalar_tensor_tensor(Bv, Bv, -1.0, A, ALU.mult, ALU.mult), reads=[lnb_], writes=[lnb_])
            for c in range(NK):
                for (t0, n) in [(0, 512), (512, 512), (1024, 128)]:
                    k.op("dve", lambda e: e.tensor_tensor(lt[:, 0:n], zT[:, c, t0:t0 + n], lnA[:, t0:t0 + n], ALU.mult), reads=[zb[c], lnb_], writes=[ltb])
                    k.op("dve", lambda e: e.tensor_tensor(lt[:, 0:n], lt[:, 0:n], lnB[:, t0:t0 + n], ALU.add), reads=[ltb, lnb_], writes=[ltb])
                    k.op("act", lambda e: e.activation(zT[:, c, t0:t0 + n], lt[:, 0:n], AF.Silu, bias=gT[:, LNB + c:LNB + c + 1], scale=gT[:, LNG + c:LNG + c + 1]),
                         reads=[ltb, c2], writes=[zb[c]])
            for cb in range(D // WC):
                tw, bw = wload(w_conv_out, cb * WC, b_conv_out)
                tg, bg = wload(w_in, C_GC + cb * WC, b_in)
                for j in range(WC // 128):
                    ch = cb * (WC // 128) + j
                    for (t0, n) in OGROUPS:
                        z0 = t0 - TH
                        b1 = bank()
                        pairs = [(tw[:, kk, j * 128:(j + 1) * 128], zT[:, kk, z0:z0 + n]) for kk in range(NK)]
                        pairs.append((tw[0:1, NK, j * 128:(j + 1) * 128], ones_b[0:1, 0:n]))
                        k.op("pe", mm(b1, psum[:, b1, 0:n], pairs), reads=[bw, c2] + zb, writes=[pbuf[b1]])
                        b2 = proj_fm(tg, bg, j, t0, n)
                        k.op("act", lambda e: e.activation(lt[:, 0:n], psum[:, b2, 0:n], AF.Sigmoid), reads=[pbuf[b2]], writes=[ltb])
                        k.op("dve", lambda e: e.tensor_tensor(mergedT[:, ch, z0:z0 + n], psum[:, b1, 0:n], lt[:, 0:n], ALU.mult),
                             reads=[pbuf[b1], ltb], writes=[mgb[ch]])
            k.barrier()
        cv.close()
        dump("mg1", mergedT[:], mgb)

        at = ExitStack()
        attnT = sb([128, NK, NOWN], BF16, "attnT", at)
        atb = [k.buf(f"at{c}") for c in range(NK)]
        with ExitStack() as s1:
            qT = sb([128, 4, NOWN], BF16, "qT", s1)
            qb = k.buf("qT")
            kTp = sb([128, 2, NT], BF16, "kTp", s1)
            kTb = k.buf("kTp")
            vP = sb([128, 10, 2, 128], BF16, "vP", s1)
            vPb = k.buf("vP")
            wkp = sb([128, NK + 1, 2, 128], BF16, "wkp", s1)
            wkb = k.buf("wkp")
            wv = sb([128, NK + 1, 64], BF16, "wv", s1)
            wvb = k.buf("wv")
            ckf = sb([128, NB, 64], F32, "ckf", s1)
            ckb = k.buf("ckf")
            cvf = sb([128, NB, 64], F32, "cvf", s1)
            cvb = k.buf("cvf")
            qS = sb([128, NB, 32], BF16, "qS", s1)
            qSb = k.buf("qS")
            ck2 = [sb([128, 128], F32, f"ck2{i}", s1) for i in range(2)]
            ck2b = [k.buf(f"ck2{i}") for i in range(2)]
            kcp = [sb([128, 2, 128], BF16, f"kcp{i}", s1) for i in range(2)]
            kcb = [k.buf(f"kcp{i}") for i in range(2)]
            vcp = [sb([128, 2, 128], BF16, f"vcp{i}", s1) for i in range(2)]
            vcb = [k.buf(f"vcp{i}") for i in range(2)]
            pbf = [sb([128, 4, 256], BF16, f"pbf{i}", s1) for i in range(2)]
            pbfb = [k.buf(f"pbf{i}") for i in range(2)]
            pT = [sb([128, 8, 128], BF16, f"pT{i}", s1) for i in range(2)]
            pTb = [k.buf(f"pT{i}") for i in range(2)]
            sm = [sb([128, 32], F32, f"sm{i}", s1) for i in range(2)]
            smb = [k.buf(f"sm{i}") for i in range(2)]
            mkc = sb([32, 128], F32, "mkc", s1)
            mkw = sb([32, 248], F32, "mkw", s1)
            sks = sb([32, 8], F32, "sks", s1)
            mksb = k.buf("mks")
            k.dma("sp", mkc[:], maskc_in, mksb, writes=[mksb])
            k.dma("sp", mkw[:], maskw_in, mksb, writes=[mksb], join=True)
            k.dma("sp", sks[:], sinks_s_in, mksb, writes=[mksb], join=True)
            k.op("dve", lambda e: e.memset(kTp[:], 0.0), writes=[kTb])
            k.op("dve", lambda e: e.memset(vP[:], 0.0), writes=[vPb])
            k.op("dve", lambda e: e.memset(wkp[:], 0.0), writes=[wkb])
            for p_ in range(2):
                k.op("dve", lambda e: e.memset(kcp[p_][:], 0.0), writes=[kcb[p_]])
                k.op("dve", lambda e: e.memset(vcp[p_][:], 0.0), writes=[vcb[p_]])
            pT_ps = psum[:].bitcast(BF16)

            def run_interleaved(gens):
                active = list(gens)
                while active:
                    for g_ in list(active):
                        try:
                            next(g_)
                        except StopIteration:
                            active.remove(g_)

            def softmax_block(np_, bsc, nh, maskap, sink_ap, par):
                S_ = psum[0:np_, bsc:bsc + 2, :].rearrange("p a (h s) -> p (a h) s", s=256)[:, 0:nh, :]
                pb2 = [pbuf[bsc], pbuf[bsc + 1]]
                smp, smpb = sm[par], smb[par]
                for (c0_, c1_, map_) in maskap:
                    k.op("dve", lambda e: e.tensor_tensor(S_[:, :, c0_:c1_], S_[:, :, c0_:c1_], map_, ALU.add), reads=pb2 + [cst, mksb], writes=pb2)
                    yield
                mx = smp[0:np_, 0:nh]
                k.op("dve", lambda e: e.reduce_max(mx, S_, AX.X), reads=pb2, writes=[smpb])
                yield
                k.op("dve", lambda e: e.tensor_tensor(mx, mx, sink_ap, ALU.max), reads=[smpb, cst, mksb], writes=[smpb])
                yield
                es = smp[0:np_, 8:8 + nh]
                k.op("dve", lambda e: e.tensor_tensor(es, sink_ap, mx, ALU.subtract), reads=[smpb, cst, mksb], writes=[smpb])
                yield
                k.op("dve", lambda e: e.tensor_tensor(S_, S_, mx.unsqueeze(2).to_broadcast([np_, nh, 256]), ALU.subtract), reads=pb2 + [smpb], writes=pb2)
                yield
                k.op("act", lambda e: e.activation(es, es, AF.Exp), reads=[smpb], writes=[smpb])
                yield
                k.op("act", lambda e: e.activation(S_, S_, AF.Exp), reads=pb2, writes=pb2)
                yield
                den = smp[0:np_, 16:16 + nh]
                k.op("dve", lambda e: e.reduce_sum(den, S_, AX.X), reads=pb2, writes=[smpb])
                yield
                k.op("dve", lambda e: e.tensor_tensor(den, den, es, ALU.add), reads=[smpb], writes=[smpb])
                yield
                k.op("dve", lambda e: e.reciprocal(den, den), reads=[smpb], writes=[smpb])
                yield
                k.op("dve", lambda e: e.tensor_tensor(pbf[par][0:np_, 0:nh, :], S_, den.unsqueeze(2).to_broadcast([np_, nh, 256]), ALU.mult),
                     reads=pb2 + [smpb], writes=[pbfb[par]])
                yield

            def prompt_step(g, n_, cp, par):
                bsc, bt, bo = 2 * par, 4 + par, 6 + par
                for hh in range(4):
                    cl = cp * 2 + hh // 2
                    eo = hh % 2
                    k.op("pe", lambda e: e.matmul(psum[:, bsc + hh // 2, (hh % 2) * 256:(hh % 2) * 256 + 256], qT[:, cl, n_ * 128:(n_ + 1) * 128],
                                                   kTp[:, eo, n_ * 128:n_ * 128 + 256], start=True, stop=True),
                         reads=[qb, kTb], writes=[pbuf[bsc + hh // 2]])
                yield
                mk = [(0, 256, (mk0 if n_ == 0 else mk1)[:].unsqueeze(1).to_broadcast([128, 4, 256]))]
                h0 = g * 8 + cp * 4
                yield from softmax_block(128, bsc, 4, mk, sinks[:, h0:h0 + 4], par)

                def trp(e):
                    ins = None
                    for hh in range(4):
                        for kb in range(2):
                            ins = e.transpose(pT_ps[:, bt, (hh * 2 + kb) * 128:(hh * 2 + kb + 1) * 128], pbf[par][:, hh, kb * 128:(kb + 1) * 128], idb[:])
                    return ins
                k.op("pe", trp, reads=[pbfb[par], c2], writes=[pbuf[bt]])
                yield
                k.op("act", lambda e: e.copy(pT[par][:].rearrange("p a t -> p (a t)"), pT_ps[:, bt, :]), reads=[pbuf[bt]], writes=[pTb[par]])
                yield
                for c2_ in range(2):
                    def pv(e):
                        ins = None
                        for eo in range(2):
                            for kb in range(2):
                                hh = c2_ * 2 + eo
                                ins = e.matmul(psum[:, bo, c2_ * 128:(c2_ + 1) * 128], vP[:, n_ + kb, eo, :], pT[par][:, hh * 2 + kb, :],
                                               start=(eo == 0 and kb == 0), stop=(eo == 1 and kb == 1))
                        return ins
                    k.op("pe", pv, reads=[vPb, pTb[par]], writes=[pbuf[bo]])
                yield
                ch0 = g * 4 + cp * 2
                k.op("act", lambda e: e.copy(attnT[:, ch0:ch0 + 2, n_ * 128:(n_ + 1) * 128], psum[:, bo, 0:256].rearrange("p (c t) -> p c t", c=2)),
                     reads=[pbuf[bo]], writes=[atb[ch0], atb[ch0 + 1]])
                yield

            def sample_step(g, bi, par):
                bsc, bt, bo = 2 * par, 4 + par, 6 + par
                ck2_, kcp_, vcp_ = ck2[par], kcp[par], vcp[par]
                k.op("act", lambda e: e.copy(ck2_[:, 0:64], ckf[:, bi, :]), reads=[ckb], writes=[ck2b[par]])
                k.op("act", lambda e: e.copy(ck2_[:, 64:128], ckf[:, bi, :]), reads=[ckb], writes=[ck2b[par]])
                yield
                k.op("dve", lambda e: e.tensor_copy(vcp_[:, 0, 0:64], cvf[:, bi, :]), reads=[cvb], writes=[vcb[par]])
                k.op("dve", lambda e: e.tensor_copy(vcp_[:, 1, 64:128], cvf[:, bi, :]), reads=[cvb], writes=[vcb[par]])
                yield
                k.op("pe", lambda e: e.transpose(psum[:, bo, 0:128], ck2_[:], idf[:]), reads=[ck2b[par], cst], writes=[pbuf[bo]])
                yield
                k.op("act", lambda e: e.copy(kcp_[0:64, 0, :], psum[0:64, bo, 0:128]), reads=[pbuf[bo]], writes=[kcb[par]])
                k.op("act", lambda e: e.copy(kcp_[64:128, 1, :], psum[64:128, bo, 0:128]), reads=[pbuf[bo]], writes=[kcb[par]])
                yield
                s0_ = TP + bi * 8
                for eo in range(2):
                    lq = qS[:, bi, :]
                    k.op("pe", lambda e: e.matmul(psum[0:32, bsc, eo * 256:eo * 256 + 128], lq, kcp_[:, eo, :], start=True, stop=True),
                         reads=[qSb, kcb[par]], writes=[pbuf[bsc]])
                    k.op("pe", lambda e: e.matmul(psum[0:32, bsc, eo * 256 + 128:eo * 256 + 256], lq, kTp[:, eo, TH + TP:NT], start=True, stop=True),
                         reads=[qSb, kTb], writes=[pbuf[bsc]])
                yield
                yield from softmax_block(32, bsc, 2, [(0, 128, mkc[:].unsqueeze(1).to_broadcast([32, 2, 128])),
                                                      (128, 256, mkw[:, 120 - 8 * bi:248 - 8 * bi].unsqueeze(1).to_broadcast([32, 2, 128]))], sks[:, g * 2:g * 2 + 2], par)

                def trs(e):
                    ins = None
                    for eo in range(2):
                        for kb in range(2):
                            ins = e.transpose(pT_ps[:, bt, (eo * 2 + kb) * 128:(eo * 2 + kb) * 128 + 32], pbf[par][0:32, eo, kb * 128:(kb + 1) * 128], idb[0:32, 0:32])
                    return ins
                k.op("pe", trs, reads=[pbfb[par], c2], writes=[pbuf[bt]])
                yield
                k.op("act", lambda e: e.copy(pT[par][:, 0:4, 0:32], pT_ps[:, bt, 0:512].rearrange("p (a t) -> p a t", t=128)[:, :, 0:32]), reads=[pbuf[bt]], writes=[pTb[par]])
                yield
                for cl in range(4):
                    def pvs(e):
                        ins = None
                        for eo in range(2):
                            ins = e.matmul(psum[:, bo, cl * 8:(cl + 1) * 8], vcp_[:, eo, :], pT[par][:, eo * 2 + 0, cl * 8:(cl + 1) * 8], start=(eo == 0), stop=False)
                            ins = e.matmul(psum[:, bo, cl * 8:(cl + 1) * 8], vP[:, 9, eo, :], pT[par][:, eo * 2 + 1, cl * 8:(cl + 1) * 8], start=False, stop=(eo == 1))
                        return ins
                    k.op("pe", pvs, reads=[vcb[par], vPb, pTb[par]], writes=[pbuf[bo]])
                yield
                ch0 = g * 4
                k.op("act", lambda e: e.copy(attnT[:, ch0:ch0 + 4, s0_:s0_ + 8], psum[:, bo, 0:32].rearrange("p (c l) -> p c l", l=8)),
                     reads=[pbuf[bo]], writes=[atb[ch0 + i_] for i_ in range(4)])
                yield

            for g in range(4):
                for half in range(2):
                    k.dma("pool", wkp[:, 0:NK, half, half * 64:(half + 1) * 64],
                          w_in.rearrange("(k p) c -> p k c", p=128)[:, :, C_K + g * 64:C_K + (g + 1) * 64], wkb, writes=[wkb], join=(half == 1))
                    k.dma("pool", wkp[0:1, NK, half, half * 64:(half + 1) * 64], b_in[0:1, C_K + g * 64:C_K + (g + 1) * 64], wkb, writes=[wkb], join=True)
                k.dma("pool", wv[:, 0:NK, :], w_in.rearrange("(k p) c -> p k c", p=128)[:, :, C_V + g * 64:C_V + (g + 1) * 64], wvb, writes=[wvb])
                k.dma("pool", wv[0:1, NK, :], b_in[0:1, C_V + g * 64:C_V + (g + 1) * 64], wvb, writes=[wvb], join=True)
                for half in range(2):
                    for (t0, n) in GROUPS:
                        b = bank()
                        pairs = [(wkp[:, kk, half, :], hT[:, kk, t0:t0 + n]) for kk in range(NK)]
                        pairs.append((wkp[0:1, NK, half, :], ones_b[0:1, 0:n]))
                        k.op("pe", mm(b, psum[:, b, 0:n], pairs), reads=[wkb, c2] + hgrp_bufs(t0, n), writes=[pbuf[b]])
                        k.op("act", lambda e: e.copy(kTp[:, half, t0:t0 + n], psum[:, b, 0:n]), reads=[pbuf[b]], writes=[kTb])
                for i in range(10):
                    b = bank()
                    pairs = [(hT[:, kk, i * 128:(i + 1) * 128], wv[:, kk, :]) for kk in range(NK)]
                    pairs.append((ones_b[0:1, 0:128], wv[0:1, NK, :]))
                    k.op("pe", mm(b, psum[:, b, 0:64], pairs), reads=[wvb, c2, hb[i]], writes=[pbuf[b]])
                    k.op("act", lambda e: e.copy(vP[:, i, 0, 0:64], psum[:, b, 0:64]), reads=[pbuf[b]], writes=[vPb])
                    k.op("act", lambda e: e.copy(vP[:, i, 1, 64:128], psum[:, b, 0:64]), reads=[pbuf[b]], writes=[vPb])
                for cc in range(2):
                    tq, bq = wload(w_in, C_Q + g * 512 + cc * WC, b_in)
                    for j in range(2):
                        for (t0, n) in OGROUPS:
                            b = proj_fm(tq, bq, j, t0, n)
                            k.op("act", lambda e: e.activation(qT[:, cc * 2 + j, t0 - TH:t0 - TH + n], psum[:, b, 0:n], AF.Copy, scale=0.125),
                                 reads=[pbuf[b]], writes=[qb])
                steps = [(n_, cp) for n_ in range(8) for cp in range(2)]
                for i_ in range(0, len(steps), 2):
                    run_interleaved([prompt_step(g, steps[i_ + p_][0], steps[i_ + p_][1], p_) for p_ in range(2)])
                k.dma("sp", ckf[:], cache_k.rearrange("b s d -> s b d")[:, :, g * 64:(g + 1) * 64], ckb, writes=[ckb])
                k.dma("sp", cvf[:], cache_v.rearrange("b s d -> s b d")[:, :, g * 64:(g + 1) * 64], cvb, writes=[cvb])
                k.op("dve", lambda e: e.tensor_copy(qS[:].rearrange("p b (c l) -> p b c l", c=4), qT[:, :, TP:NOWN].rearrange("p c (b l) -> p b c l", l=8)),
                     reads=[qb], writes=[qSb])
                for bi in range(0, NB, 2):
                    run_interleaved([sample_step(g, bi + p_, p_) for p_ in range(2)])
            k.barrier()
        dump("attnT", attnT[:], atb)

        with ExitStack() as s1:
            lt = sb([128, 512], F32, "lt2", s1)
            ltb = k.buf("lt2")
            l2 = sb([128, 512], F32, "l2", s1)
            l2b = k.buf("l2")
            for cb in range(D // WC):
                tw, bw = wload(w_attn_out, cb * WC, b_attn_out)
                tg, bg = wload(w_in, C_GA + cb * WC, b_in)
                for j in range(WC // 128):
                    ch = cb * (WC // 128) + j
                    for (t0, n) in OGROUPS:
                        z0 = t0 - TH
                        b1 = bank()
                        pairs = [(tw[:, kk, j * 128:(j + 1) * 128], attnT[:, kk, z0:z0 + n]) for kk in range(NK)]
                        pairs.append((tw[0:1, NK, j * 128:(j + 1) * 128], ones_b[0:1, 0:n]))
                        k.op("pe", mm(b1, psum[:, b1, 0:n], pairs), reads=[bw, c2] + atb, writes=[pbuf[b1]])
                        b2 = proj_fm(tg, bg, j, t0, n)
                        k.op("act", lambda e: e.activation(lt[:, 0:n], psum[:, b2, 0:n], AF.Sigmoid), reads=[pbuf[b2]], writes=[ltb])
                        k.op("dve", lambda e: e.tensor_tensor(l2[:, 0:n], psum[:, b1, 0:n], lt[:, 0:n], ALU.mult), reads=[pbuf[b1], ltb], writes=[l2b])
                        k.op("dve", lambda e: e.tensor_tensor(mergedT[:, ch, z0:z0 + n], l2[:, 0:n], mergedT[:, ch, z0:z0 + n], ALU.add),
                             reads=[l2b, mgb[ch]], writes=[mgb[ch]])
            k.barrier()
        at.close()
        dump("mg2", mergedT[:], mgb)

        x1b = [k.buf(f"x1_{i}") for i in range(9)]
        with ExitStack() as s1:
            mtok = sb([NR, 2, D], F32, "mtok2", s1)
            mtokb = k.buf("mtok2")
            k.dma("sp", mtok[:], mtok_d, mtokb, reads=[mtokd_b], writes=[mtokb])
            m2 = sb([128, 2, WC], F32, "m2", s1)
            m2b = k.buf("m2")
            xc = [sb([128, WC], F32, f"xc{i}", s1) for i in range(2)]
            xcb = [k.buf(f"xc{i}") for i in range(2)]
            t1 = [sb([128, WC], F32, f"t1{i}", s1) for i in range(2)]
            t1b = [k.buf(f"t1{i}") for i in range(2)]
            it = 0
            for cb in range(D // WC):
                tw, bw = wload(w_out, cb * WC, None)
                cols = slice(cb * WC, (cb + 1) * WC)
                b = bank()

                def selmm(e):
                    e.matmul(psum[:, b, 0:WC], sel[:, 0:128], mtok[:, 0, cols], start=True, stop=True)
                    return e.matmul(psum[:, b, WC:2 * WC], sel[:, 128:256], mtok[:, 0, cols], start=True, stop=True)
                k.op("pe", selmm, reads=[cst, mtokb], writes=[pbuf[b]])
                k.op("act", lambda e: e.copy(m2[:].rearrange("p a c -> p (a c)"), psum[:, b, 0:2 * WC]), reads=[pbuf[b]], writes=[m2b])
                for i in range(9):
                    r = it % 2
                    it += 1
                    b = bank()
                    pairs = [(mergedT[:, kk, i * 128:(i + 1) * 128], tw[:, kk, :]) for kk in range(NK)]
                    k.op("pe", mm(b, psum[:, b, 0:WC], pairs), reads=[bw] + mgb, writes=[pbuf[b]])
                    k.dma("sp", xc[r][:], xin[TH + i * 128:TH + (i + 1) * 128, cols], xcb[r], writes=[xcb[r]])
                    k.op("dve", lambda e: e.tensor_tensor(t1[r][:], psum[:, b, 0:WC], m2[:, 1 if i == 8 else 0, :], ALU.mult), reads=[pbuf[b], m2b], writes=[t1b[r]])
                    k.op("dve", lambda e: e.tensor_tensor(t1[r][:], t1[r][:], xc[r][:], ALU.add), reads=[t1b[r], xcb[r]], writes=[t1b[r]])
                    k.dma("sp", x1_d[i * 128:(i + 1) * 128, cols], t1[r][:], t1b[r], reads=[t1b[r]], writes=[x1b[i]], join=True)
            m5 = sb([128, D], F32, "m5", s1)
            m5b = k.buf("m5")
            for j2 in range(2):
                for cg in range(4):
                    b = bank()
                    k.op("pe", lambda e: e.matmul(psum[:, b, 0:512], sel[:, j2 * 128:(j2 + 1) * 128], mtok[:, 1, cg * 512:(cg + 1) * 512], start=True, stop=True),
                         reads=[cst, mtokb], writes=[pbuf[b]])
                    k.op("act", lambda e: e.copy(m5[:, cg * 512:(cg + 1) * 512], psum[:, b, 0:512]), reads=[pbuf[b]], writes=[m5b])
                k.dma("sp", m5_d[j2], m5[:], m5b, reads=[m5b], writes=[x1b[0]], join=True)
            k.barrier()

        with ExitStack() as s1:
            wr = sb([128, NK, NE], F32, "wr", s1)
            br = sb([1, NE], F32, "br", s1)
            wrb = k.buf("wr")
            k.dma("sp", wr[:], w_router.rearrange("(k p) e -> p k e", p=128), wrb, writes=[wrb])
            k.dma("sp", br[:], b_router, wrb, writes=[wrb], join=True)
            h2f = sb([128, NK, 128], F32, "h2f", s1)
            h2fb = k.buf("h2f")
            rt = sb([128, 4, NE], F32, "rt", s1)
            rtb = k.buf("rt")
            xts, xtb, junk, junkb, stat, statb, mtmp, mtmpb = norm_alloc(s1)
            for i in range(9):
                xt, xb = xts[i % 2], xtb[i % 2]
                k.dma("sp", xt[:], x1_d[i * 128:(i + 1) * 128, :], xb, reads=[x1b[i]], writes=[xb])
                norm_mod_T(xt, xb, S2T, B2T, i == 8, lambda kk0, i=i: h2T[:, kk0:kk0 + 4, i * 128:(i + 1) * 128], [h2b[i]], f32_dst=(h2f, h2fb))
                b = bank()
                pairs = [(h2f[:, kk, :], wr[:, kk, :]) for kk in range(NK)]
                pairs.append((ones_f[0:1, 0:128], br[0:1, :]))
                k.op("pe", mm(b, psum[:, b, 0:NE], pairs), reads=[h2fb, wrb, c2], writes=[pbuf[b]])
                lg = rt[:, 0, :]
                k.op("act", lambda e: e.copy(lg, psum[:, b, 0:NE]), reads=[pbuf[b]], writes=[rtb])
                m8 = rt[:, 1, 0:8]
                k.op("dve", lambda e: e.max(m8, lg), reads=[rtb], writes=[rtb])
                msk = rt[:, 2, :]
                k.op("dve", lambda e: e.tensor_scalar(msk, lg, rt[:, 1, 3:4], 0.0, ALU.is_ge, ALU.add), reads=[rtb], writes=[rtb])
                k.op("dve", lambda e: e.tensor_scalar(rt[:, 1, 8:9], rt[:, 1, 0:1], -1.0, None, ALU.mult), reads=[rtb], writes=[rtb])
                ex = rt[:, 3, :]
                k.op("act", lambda e: e.activation(ex, lg, AF.Exp, bias=rt[:, 1, 8:9]), reads=[rtb], writes=[rtb])
                k.op("dve", lambda e: e.tensor_tensor(ex, ex, msk, ALU.mult), reads=[rtb], writes=[rtb])
                k.op("dve", lambda e: e.reduce_sum(rt[:, 1, 9:10], ex, AX.X), reads=[rtb], writes=[rtb])
                k.op("dve", lambda e: e.reciprocal(rt[:, 1, 10:11], rt[:, 1, 9:10]), reads=[rtb], writes=[rtb])
                k.op("dve", lambda e: e.tensor_scalar(comb[:, i, :], ex, rt[:, 1, 10:11], 0.0, ALU.mult, ALU.add), reads=[rtb], writes=[combb])
            k.barrier()
        dump("h2T", h2T[:], h2b)
        dump("comb", comb[:], [combb])
        if stage < 5:
            if "x1" in dbg_names:
                o = dout("dbg_x1", [NOWN, D])
                k.dma("sp", o, x1_d, dbgq, reads=x1b, store=True)
            k.barrier()
            mix.close()
            k.finish()
            return nc
        k.barrier()
        mix.close()

        acc = sb([128, 9, D], F32, "acc")
        accb = [k.buf(f"acc{i}") for i in range(9)]
        with ExitStack() as s1:
            actT = sb([128, NK, NOWN], BF16, "actT", s1)
            actb = [k.buf(f"act{f}") for f in range(NK)]
            b2 = sb([NE, D], F32, "b2", s1)
            b2b = k.buf("b2")
            k.dma("sp", b2[:], b_mlp2, b2b, writes=[b2b])
            combT = sb([NE, 9, 128], F32, "combT", s1)
            cTb = k.buf("combT")
            tA = sb([128, 512], F32, "tA", s1)
            tB = sb([128, 512], F32, "tB", s1)
            tC = sb([128, 512], F32, "tC", s1)
            tAb, tBb, tCb = k.buf("tA"), k.buf("tB"), k.buf("tC")
            for i in range(9):
                b = bank()
                k.op("pe", lambda e: e.transpose(psum[0:NE, b, 0:128], comb[:, i, :], idf[:]), reads=[combb, cst], writes=[pbuf[b]])
                k.op("act", lambda e: e.copy(combT[:, i, :], psum[0:NE, b, 0:128]), reads=[pbuf[b]], writes=[cTb])
            for i in range(9):
                for cg in range(4):
                    b = bank()
                    k.op("pe", lambda e: e.matmul(psum[:, b, 0:512], combT[:, i, :], b2[:, cg * 512:(cg + 1) * 512], start=True, stop=True),
                         reads=[cTb, b2b], writes=[pbuf[b]])
                    k.op("act", lambda e: e.copy(acc[:, i, cg * 512:(cg + 1) * 512], psum[:, b, 0:512]), reads=[pbuf[b]], writes=[accb[i]])
            MG = [(0, 512), (512, 512), (1024, 128)]
            for ex_ in range(n_exp):
                for f in range(NK):
                    t1_, bf1 = wload(w_mlp1[ex_], f * WC, b_mlp1[ex_])
                    for (t0, n) in MG:
                        bg_ = bank()
                        bl_ = bank()
                        hbs = [h2b[i] for i in range(t0 // 128, (t0 + n) // 128)]
                        for (bb_, off) in ((bg_, 0), (bl_, 1)):
                            pairs = [(t1_[:, kk, off:WC:2], h2T[:, kk, t0:t0 + n]) for kk in range(NK)]
                            pairs.append((t1_[0:1, NK, off:WC:2], ones_b[0:1, 0:n]))
                            k.op("pe", mm(bb_, psum[:, bb_, 0:n], pairs), reads=[bf1, c2] + hbs, writes=[pbuf[bb_]])
                        k.op("dve", lambda e: e.tensor_scalar(tA[:, 0:n], psum[:, bg_, 0:n], 7.0, None, ALU.min), reads=[pbuf[bg_]], writes=[tAb])
                        k.op("act", lambda e: e.activation(tB[:, 0:n], tA[:, 0:n], AF.Sigmoid, scale=1.702), reads=[tAb], writes=[tBb])
                        k.op("dve", lambda e: e.tensor_scalar(tC[:, 0:n], psum[:, bl_, 0:n], 7.0, -7.0, ALU.min, ALU.max), reads=[pbuf[bl_]], writes=[tCb])
                        k.op("dve", lambda e: e.scalar_tensor_tensor(tC[:, 0:n], tC[:, 0:n], 1.0, tA[:, 0:n], ALU.add, ALU.mult), reads=[tCb, tAb], writes=[tCb])
                        k.op("dve", lambda e: e.tensor_tensor(actT[:, f, t0:t0 + n], tC[:, 0:n], tB[:, 0:n], ALU.mult), reads=[tCb, tBb], writes=[actb[f]])
                for db in range(D // WC):
                    t2_, bf2 = wload(w_mlp2[ex_], db * WC, None)
                    for i in range(9):
                        b = bank()
                        pairs = [(actT[:, f, i * 128:(i + 1) * 128], t2_[:, f, :]) for f in range(NK)]
                        k.op("pe", mm(b, psum[:, b, 0:WC], pairs), reads=[bf2] + actb, writes=[pbuf[b]])
                        av = acc[:, i, db * WC:(db + 1) * WC]
                        k.op("dve", lambda e: e.scalar_tensor_tensor(av, psum[:, b, 0:WC], comb[:, i, ex_:ex_ + 1], av, ALU.mult, ALU.add),
                             reads=[pbuf[b], combb, accb[i]], writes=[accb[i]])
            k.barrier()

        with ExitStack() as s1:
            gf = sb([128, D], F32, "gf", s1)
            gfb = k.buf("gf")
            k.dma("sp", gf[:], normf_bc, gfb, writes=[gfb])
            m5t = [sb([128, D], F32, f"m5t{i}", s1) for i in range(2)]
            m5tb = k.buf("m5t")
            k.dma("sp", m5t[0][:], m5_d[0], m5tb, reads=[x1b[0]], writes=[m5tb])
            k.dma("sp", m5t[1][:], m5_d[1], m5tb, reads=[x1b[0]], writes=[m5tb], join=True)
            xf = [sb([128, D], F32, f"xf{i}", s1) for i in range(2)]
            xfb = [k.buf(f"xf{i}") for i in range(2)]
            junk2 = sb([128, D], BF16, "junk2", s1)
            j2b = k.buf("junk2")
            st2 = sb([128, 8], F32, "st2", s1)
            st2b = k.buf("st2")
            for i in range(9):
                xt, xb = xf[i % 2], xfb[i % 2]
                k.dma("sp", xt[:], x1_d[i * 128:(i + 1) * 128, :], xb, reads=[x1b[i]], writes=[xb])
                mt = m5t[1 if i == 8 else 0]
                k.op("dve", lambda e: e.tensor_tensor(acc[:, i, :], acc[:, i, :], mt[:], ALU.mult), reads=[accb[i], m5tb], writes=[accb[i]])
                k.op("dve", lambda e: e.tensor_tensor(xt[:], xt[:], acc[:, i, :], ALU.add), reads=[xb, accb[i]], writes=[xb])
                k.op("act", lambda e: e.activation(junk2[:], xt[:], AF.Square, accum_out=st2[:, 0:1]), reads=[xb], writes=[j2b, st2b])
                k.op("dve", lambda e: e.tensor_scalar(st2[:, 1:2], st2[:, 0:1], 1.0 / D, EPS, ALU.mult, ALU.add), reads=[st2b], writes=[st2b])
                k.op("act", lambda e: e.activation(st2[:, 3:4], st2[:, 1:2], AF.Sqrt), reads=[st2b], writes=[st2b])
                k.op("dve", lambda e: e.reciprocal(st2[:, 2:3], st2[:, 3:4]), reads=[st2b], writes=[st2b])
                k.op("dve", lambda e: e.scalar_tensor_tensor(xt[:], xt[:], st2[:, 2:3], gf[:], ALU.mult, ALU.mult), reads=[xb, st2b, gfb], writes=[xb])
                k.dma("sp", y_out[i * 128:(i + 1) * 128, :], xt[:], xb, reads=[xb], store=True)
            k.barrier()
        k.finish()
    return nc

STAGE = 9
NEG = -30000.0


def _core_inputs(c, I, stage, n_exp=NE):
    pb, half = c // 2, c % 2
    f = np.float32
    xp = I["x_prompt"][pb]
    own = xp[half * TP:(half + 1) * TP]
    halo = xp[half * TP - TH:half * TP] if half == 1 else np.zeros((TH, D), f)
    xs = I["x_sample"][NB * c:NB * (c + 1)].reshape(TS, D)
    i = np.arange(128)[:, None]
    j = np.arange(256)[None, :]
    diff = i + 128 - j
    band = (diff >= 0) & (diff < 128)
    maskp = np.where(band, 0.0, NEG).astype(f)
    maskp0 = maskp if half == 1 else np.where(band & (j >= 128), 0.0, NEG).astype(f)
    l = (np.arange(32) % 8)[:, None]
    maskc = np.where(np.arange(128)[None, :] > l, 0.0, NEG).astype(f)
    xw = np.arange(248)[None, :] - 120
    maskw = np.where((xw >= 0) & (xw <= l), 0.0, NEG).astype(f)
    sel = np.zeros((NR, 256), f)
    sel[0, 0:128] = 1.0
    for b in range(NB):
        sel[1 + b, 128 + 8 * b:128 + 8 * b + 8] = 1.0
    sk = I["sinks"]
    sinks_s = np.zeros((32, 8), f)
    for r in range(32):
        for g in range(4):
            for eo in range(2):
                sinks_s[r, g * 2 + eo] = sk[g * 8 + (r // 8) * 2 + eo]
    m = {
        "xin": np.concatenate([halo, own, xs], 0),
        "c_own": np.concatenate([I["c_prompt"][pb:pb + 1], I["c_sample"][NB * c:NB * (c + 1)], np.zeros((NR - 17, D), f)], 0),
        "cache_k": I["cache_k"][NB * c:NB * (c + 1)].reshape(NB, 128, 256),
        "cache_v": I["cache_v"][NB * c:NB * (c + 1)].reshape(NB, 128, 256),
        "state_conv": I["state_conv"][NB * c:NB * (c + 1)],
        "w_ada": I["w_ada"], "b_ada": I["b_ada"].reshape(1, -1),
        "w_in": I["w_in"], "b_in": I["b_in"].reshape(1, -1),
        "vec5": np.concatenate([I["norm1_g"], I["conv_dw_b"], I["conv_ln_g"], I["conv_ln_b"], I["norm2_g"]]).reshape(80, 128),
        "conv_dw_w": I["conv_dw_w"],
        "w_conv_out": I["w_conv_out"], "b_conv_out": I["b_conv_out"].reshape(1, -1),
        "sinks_bc": np.broadcast_to(sk[None, :], (128, 32)),
        "w_attn_out": I["w_attn_out"], "b_attn_out": I["b_attn_out"].reshape(1, -1),
        "w_out": I["w_out"], "w_router": I["w_router"], "b_router": I["b_router"].reshape(1, -1),
        "normf_bc": np.broadcast_to(I["norm_f_g"][None, :], (128, D)),
        "ident": np.eye(128, dtype=f), "maskp0": maskp0, "maskp": maskp, "maskc": maskc, "maskw": maskw,
        "hv": np.full((128, 1), float(half), f), "sinks_s": sinks_s, "sel": sel,
    }
    if stage >= 5:
        m["w_mlp1"] = I["w_mlp1"][:n_exp]
        m["b_mlp1"] = I["b_mlp1"][:n_exp].reshape(n_exp, 1, 2 * D)
        m["w_mlp2"] = I["w_mlp2"][:n_exp]
        m["b_mlp2"] = I["b_mlp2"]
    return {k_: np.ascontiguousarray(v, dtype=f) for k_, v in m.items()}


def _run(I, stage, cores, dbg_names=(), n_exp=NE):
    nc = build(stage=stage, n_exp=n_exp, dbg_names=dbg_names)
    in_maps = [_core_inputs(c, I, stage, n_exp) for c in cores]
    res = run_bass_kernel_spmd(nc, in_maps, core_ids=list(range(len(cores))))
    return res.results


def kernel(**I):
    I = {k_: np.asarray(v) for k_, v in I.items()}
    R = _run(I, STAGE, list(range(8)))
    f = np.float32
    y_p = np.zeros((4, 2048, D), f)
    y_s = np.zeros((128, 8, D), f)
    k_p = np.zeros((4, 128, 4, 64), f)
    v_p = np.zeros((4, 128, 4, 64), f)
    cv_p = np.zeros((4, 30, D), f)
    k_s = np.zeros((128, 128, 4, 64), f)
    v_s = np.zeros((128, 128, 4, 64), f)
    cv_s = np.zeros((128, 30, D), f)
    for c in range(8):
        r = R[c]
        pb, half = c // 2, c % 2
        y_p[pb, half * TP:(half + 1) * TP] = r["y"][0:TP]
        y_s[NB * c:NB * (c + 1)] = r["y"][TP:].reshape(NB, 8, D)
        if half == 1:
            k_p[pb] = r["klast"].reshape(128, 4, 64)
            v_p[pb] = r["vlast"].reshape(128, 4, 64)
            cv_p[pb] = r["convlast"]
        k_s[NB * c:NB * (c + 1)] = r["ks_out"].reshape(NB, 128, 4, 64)
        v_s[NB * c:NB * (c + 1)] = r["vs_out"].reshape(NB, 128, 4, 64)
        cv_s[NB * c:NB * (c + 1)] = r["convs_out"]
    return (y_p, y_s, k_p, v_p, cv_p, k_s, v_s, cv_s)
```

```python
import numpy as np
from contextlib import ExitStack
import concourse.bass as bass
import concourse.mybir as mybir
from concourse.bass_utils import run_bass_kernel_spmd

F32 = mybir.dt.float32
BF16 = mybir.dt.bfloat16
ALU = mybir.AluOpType
AF = mybir.ActivationFunctionType
AX = mybir.AxisListType

D = 2048
NK = 16
TH, TP, TS = 128, 1024, 128
NT = TH + TP + TS
NOWN = TP + TS
NB = 16
NE = 32
MOE_CAP = 384
NR = 32
EPS = 1e-5
C_UA, C_UB, C_Q, C_K, C_V, C_GC, C_GA = 0, 2048, 4096, 6144, 6400, 6656, 8704
IN_W = 10752
GROUPS = [(0, 512), (512, 512), (1024, 256)]
OGROUPS = [(128, 512), (640, 512), (1152, 128)]


class Buf:
    __slots__ = ("name", "w", "r", "dsem", "dcnt")

    def __init__(self, name):
        self.name = name
        self.w = None
        self.r = {}
        self.dsem = None
        self.dcnt = 0


class K:
    def __init__(self, nc, st):
        self.nc = nc
        self.st = st
        self.eng = dict(pe=nc.tensor, act=nc.scalar, dve=nc.vector, pool=nc.gpsimd, sp=nc.sync)
        self.sem = {k: st.enter_context(nc.semaphore("s_" + k)) for k in self.eng}
        self.cnt = {k: 0 for k in self.eng}
        self.known = {k: {} for k in self.eng}
        self.semobj = {}
        for k, s in self.sem.items():
            self.semobj[s.num] = s
        self.nbuf = 0
        self.store_tokens = []
        self.alldma = {}
        self.dbufs = []
        self.flagregs = None

    def buf(self, name=None):
        self.nbuf += 1
        return Buf(f"{name or 'b'}_{self.nbuf}")

    def _waits(self, eng, reads, writes, skipw=False):
        waits = {}

        def need(tok):
            if tok is None:
                return
            s, v = tok
            if waits.get(s, 0) < v:
                waits[s] = v

        for b in reads:
            need(b.w)
        for b in writes:
            if not skipw:
                need(b.w)
            for s, v in b.r.items():
                need((s, v))
        kn = self.known[eng]
        e = self.eng[eng]
        for s, v in waits.items():
            if kn.get(s, 0) < v:
                kn[s] = v
                e.wait_ge(self.semobj[s], v)

    def op(self, eng, fn, reads=(), writes=()):
        self._waits(eng, reads, writes)
        ins = fn(self.eng[eng])
        sem = self.sem[eng]
        ins.then_inc(sem, 1)
        self.cnt[eng] += 1
        tok = (sem.num, self.cnt[eng])
        for b in reads:
            if b.r.get(tok[0], 0) < tok[1]:
                b.r[tok[0]] = tok[1]
        for b in writes:
            b.w = tok
            b.r = {}
        return tok

    def dma(self, eng, out, in_, owner, reads=(), writes=(), join=False, store=False):
        if owner.dsem is None:
            owner.dsem = self.st.enter_context(self.nc.semaphore("d_" + owner.name))
            self.dbufs.append(owner)
            self.semobj[owner.dsem.num] = owner.dsem
        self._waits(eng, reads, writes, skipw=join)
        self.eng[eng].dma_start(out=out, in_=in_).then_inc(owner.dsem, 16)
        owner.dcnt += 16
        tok = (owner.dsem.num, owner.dcnt)
        self.alldma[tok[0]] = tok[1]
        for b in reads:
            if b.r.get(tok[0], 0) < tok[1]:
                b.r[tok[0]] = tok[1]
        for b in writes:
            b.w = tok
            b.r = {}
        if store:
            self.store_tokens.append(tok)
        return tok

    def barrier(self):
        fin = dict(self.alldma)
        for k_ in self.eng:
            if self.cnt[k_]:
                fin[self.sem[k_].num] = self.cnt[k_]
        for eng, e in self.eng.items():
            kn = self.known[eng]
            for s_, v in fin.items():
                if kn.get(s_, 0) < v:
                    kn[s_] = v
                    e.wait_ge(self.semobj[s_], v)

    def cond_region(self, flag_ap, body):
        self.barrier()
        c0 = dict(self.cnt)
        d0 = {id(b): b.dcnt for b in self.dbufs}
        self.nreg = getattr(self, "nreg", 0) + 1
        regs = self.nc.alloc_registers(f"flagregs{self.nreg}", mybir.ALL_ENGINES)
        for reg in regs:
            self.nc.reg_load(reg, flag_ap)
        v = self.nc.snap(regs, donate=True)
        with self.nc.If(v > 0):
            body()
        with self.nc.Else():
            for eng, e in self.eng.items():
                dlt = self.cnt[eng] - c0[eng]
                if dlt:
                    e.sem_inc(self.sem[eng], dlt)
            for b in self.dbufs:
                dlt = b.dcnt - d0.get(id(b), 0)
                if dlt:
                    self.eng["sp"].sem_inc(b.dsem, dlt)
        for reg in regs:
            self.nc.free_register(reg)

    def finish(self):
        e = self.eng["sp"]
        fin = {}
        for s, v in self.store_tokens:
            fin[s] = max(fin.get(s, 0), v)
        for k in self.eng:
            if self.cnt[k]:
                fin[self.sem[k].num] = self.cnt[k]
        for s, v in fin.items():
            e.wait_ge(self.semobj[s], v)


def build(stage=9, n_exp=NE, dbg_names=()):
    nc = bass.Bass("TRN2", target_bir_lowering=False)

    def din(name, shape):
        return nc.dram_tensor(name, list(shape), F32, kind="ExternalInput").ap()

    def dout(name, shape):
        return nc.dram_tensor(name, list(shape), F32, kind="ExternalOutput").ap()

    xin = din("xin", [NT, D])
    c_own = din("c_own", [NR, D])
    cache_k = din("cache_k", [NB, 128, 256])
    cache_v = din("cache_v", [NB, 128, 256])
    state_conv = din("state_conv", [NB, 30, D])
    w_ada = din("w_ada", [D, 6 * D])
    b_ada = din("b_ada", [1, 6 * D])
    w_in = din("w_in", [D, IN_W])
    b_in = din("b_in", [1, IN_W])
    vec5 = din("vec5", [80, 128])
    conv_dw_w = din("conv_dw_w", [31, D])
    w_conv_out = din("w_conv_out", [D, D])
    b_conv_out = din("b_conv_out", [1, D])
    sinks_bc = din("sinks_bc", [128, 32])
    w_attn_out = din("w_attn_out", [D, D])
    b_attn_out = din("b_attn_out", [1, D])
    w_out = din("w_out", [D, D])
    w_router = din("w_router", [D, NE])
    b_router = din("b_router", [1, NE])
    normf_bc = din("normf_bc", [128, D])
    ident_in = din("ident", [128, 128])
    maskp0 = din("maskp0", [128, 256])
    maskp = din("maskp", [128, 256])
    maskc_in = din("maskc", [32, 128])
    maskw_in = din("maskw", [32, 248])
    hv_in = din("hv", [128, 1])
    sinks_s_in = din("sinks_s", [32, 8])
    sel_in = din("sel", [NR, 256])
    utri_in = din("utri", [128, 128])
    iotac_in = din("iotac", [128, 1152])
    iotap_in = din("iotap", [128, 9])
    if stage >= 5:
        w_mlp1 = din("w_mlp1", [n_exp, D, 2 * D])
        b_mlp1 = din("b_mlp1", [n_exp, 1, 2 * D])
        w_mlp2 = din("w_mlp2", [n_exp, D, D])
        b_mlp2 = din("b_mlp2", [n_exp, 1, D])

    y_out = dout("y", [NOWN, D])
    klast = dout("klast", [128, 256])
    vlast = dout("vlast", [128, 256])
    convlast = dout("convlast", [30, D])
    ks_out = dout("ks_out", [NB, 128, 256])
    vs_out = dout("vs_out", [NB, 128, 256])
    convs_out = dout("convs_out", [NB, 30, D])
    x1_d = nc.dram_tensor("x1_scratch", [NOWN, D], F32).ap()
    m5_d = nc.dram_tensor("m5_scratch", [2, 128, D], F32).ap()
    mtok_d = nc.dram_tensor("mtok_scratch", [NR, 2, D], F32).ap()
    h2_d = nc.dram_tensor("h2_scratch", [9, 128, D], BF16).ap()
    flags_d = nc.dram_tensor("flags_scratch", [1, 512], mybir.dt.int32).ap()

    with ExitStack() as st:
        E = st.enter_context
        k = K(nc, st)
        uid = [0]

        def sb(shape, dt, name=None, stack=st):
            uid[0] += 1
            return stack.enter_context(nc.sbuf_tensor(f"{name or 't'}{uid[0]}", list(shape), dt))

        dbgq = k.buf("dbgq")

        def dump(name, ap, bufs):
            if name not in dbg_names:
                return
            shp = list(ap.shape)
            o = dout("dbg_" + name, [shp[0], int(np.prod(shp[1:]))])
            src = ap
            if len(shp) == 3:
                o = o.rearrange("p (a b) -> p a b", a=shp[1])
            k.dma("pool", o, src, dbgq, reads=bufs, store=True)

        psum = E(nc.psum_tensor("psum", [128, 8, 512], F32))
        pbuf = [k.buf(f"ps{i}") for i in range(8)]
        prr = [0]

        def bank():
            i = prr[0] % 8
            prr[0] += 1
            return i

        def bank2():
            i = ((prr[0] + 1) // 2 * 2) % 8
            prr[0] = i + 2
            return i

        def mm(bi, out_ap, pairs):
            def fn(e):
                ins = None
                n = len(pairs)
                for j, (l, r) in enumerate(pairs):
                    ins = e.matmul(out_ap, l, r, start=(j == 0), stop=(j == n - 1))
                return ins
            return fn

        cst = k.buf("cst")
        idf = sb([128, 128], F32, "idf")
        idb = sb([128, 128], BF16, "idb")
        ones_b = sb([128, 512], BF16, "ones_b")
        ones_f = sb([32, 128], F32, "ones_f")
        k.dma("sp", idf[:], ident_in, cst, writes=[cst])
        c2 = k.buf("c2")
        k.op("dve", lambda e: e.tensor_copy(idb[:], idf[:]), reads=[cst], writes=[c2])
        k.op("dve", lambda e: e.memset(ones_b[:], 1.0), writes=[c2])
        k.op("dve", lambda e: e.memset(ones_f[:], 1.0), writes=[c2])

        NRING = 3
        WC = 256
        wring = [sb([128, NK + 1, WC], BF16, f"wr{i}") for i in range(NRING)]
        wbufs = [k.buf(f"wr{i}") for i in range(NRING)]
        wrr = [0]

        def wload(wmat, c0, brow=None, ncols=WC):
            i = wrr[0] % NRING
            wrr[0] += 1
            t, bf = wring[i], wbufs[i]
            src = wmat.rearrange("(k p) c -> p k c", p=128)[:, :, c0:c0 + ncols]
            if brow is not None:
                k.dma("pool", t[0:1, NK, 0:ncols], brow[0:1, c0:c0 + ncols], bf, writes=[bf])
            k.dma("pool", t[:, 0:NK, 0:ncols], src, bf, writes=[bf], join=(brow is not None))
            return t, bf

        comb = sb([128, 9, NE], F32, "comb")
        combb = k.buf("comb")

        mix = ExitStack()
        hT = sb([128, NK, NT], BF16, "hT", mix)
        hb = [k.buf(f"hT{i}") for i in range(10)]
        h2T = hT[:, :, 0:NOWN]
        h2b = [k.buf(f"h2T{i}") for i in range(9)]
        gT = sb([128, 80], F32, "gT", mix)
        dwT = sb([128, NK, 31], F32, "dwT", mix)
        sinks = sb([128, 32], F32, "sinks", mix)
        hv = sb([128, 1], F32, "hv", mix)
        sel = sb([NR, 256], F32, "sel", mix)
        mk0 = sb([128, 256], F32, "mk0", mix)
        mk1 = sb([128, 256], F32, "mk1", mix)
        k.dma("sp", sinks[:], sinks_bc, cst, writes=[cst], join=True)
        k.dma("sp", hv[:], hv_in, cst, writes=[cst], join=True)
        k.dma("sp", sel[:], sel_in, cst, writes=[cst], join=True)
        k.dma("sp", mk0[:], maskp0, cst, writes=[cst], join=True)
        k.dma("sp", mk1[:], maskp, cst, writes=[cst], join=True)

        with ExitStack() as s0:
            v5 = sb([80, 128], F32, "v5", s0)
            dw = sb([31, D], F32, "dw", s0)
            ld = k.buf("ld0")
            k.dma("sp", v5[:], vec5, ld, writes=[ld])
            k.dma("sp", dw[:], conv_dw_w, ld, writes=[ld], join=True)
            b = bank()
            k.op("pe", lambda e: e.transpose(psum[:, b, 0:80], v5[:], idf[0:80, 0:80]), reads=[ld, cst], writes=[pbuf[b]])
            k.op("dve", lambda e: e.tensor_copy(gT[:], psum[:, b, 0:80]), reads=[pbuf[b]], writes=[c2])
            for c in range(NK):
                b = bank()
                k.op("pe", lambda e: e.transpose(psum[:, b, 0:31], dw[:, c * 128:(c + 1) * 128], idf[0:31, 0:31]),
                     reads=[ld, cst], writes=[pbuf[b]])
                k.op("dve", lambda e: e.tensor_copy(dwT[:, c, :], psum[:, b, 0:31]), reads=[pbuf[b]], writes=[c2])
            k.barrier()
        G1, DWB, LNG, LNB, G2 = 0, 16, 32, 48, 64

        NM = 17
        modT = sb([128, 4, NK, NM], F32, "modT", mix)
        S1T = sb([128, NK, NM], F32, "S1T", mix)
        S2T = sb([128, NK, NM], F32, "S2T", mix)
        modb = k.buf("modT")
        mtokb = k.buf("mtok")
        with ExitStack() as s0:
            cs = sb([NR, D], F32, "cs", s0)
            mtok = sb([NR, 2, D], F32, "mtok", s0)
            scT = sb([128, NK, NR], BF16, "scT", s0)
            csb = k.buf("cs")
            k.dma("sp", cs[:], c_own, csb, writes=[csb])
            k.op("act", lambda e: e.activation(cs[:], cs[:], AF.Silu), reads=[csb], writes=[csb])
            scb = k.buf("scT")
            for kk in range(NK):
                b = bank()
                k.op("pe", lambda e: e.transpose(psum[:, b, 0:NR], cs[:, kk * 128:(kk + 1) * 128], idf[0:NR, 0:NR]),
                     reads=[csb, cst], writes=[pbuf[b]])
                k.op("dve", lambda e: e.tensor_copy(scT[:, kk, :], psum[:, b, 0:NR]), reads=[pbuf[b]], writes=[scb])
            fm_slot = {0: 0, 1: 1, 3: 2, 4: 3}
            for mi in range(6):
                for cb in range(D // WC):
                    c0 = mi * D + cb * WC
                    t, bf = wload(w_ada, c0, b_ada)
                    if mi in fm_slot:
                        for j in range(WC // 128):
                            b = bank()
                            pairs = [(t[:, kk, j * 128:(j + 1) * 128], scT[:, kk, :]) for kk in range(NK)]
                            pairs.append((t[0:1, NK, j * 128:(j + 1) * 128], ones_b[0:1, 0:NR]))
                            k.op("pe", mm(b, psum[:, b, 0:NR], pairs), reads=[bf, scb, c2], writes=[pbuf[b]])
                            ch = cb * (WC // 128) + j
                            k.op("act", lambda e: e.copy(modT[:, fm_slot[mi], ch, :], psum[:, b, 0:NM]),
                                 reads=[pbuf[b]], writes=[modb])
                    else:
                        b = bank()
                        pairs = [(scT[:, kk, :], t[:, kk, :]) for kk in range(NK)]
                        pairs.append((ones_b[0:1, 0:NR], t[0:1, NK, :]))
                        k.op("pe", mm(b, psum[0:NR, b, 0:WC], pairs), reads=[bf, scb, c2], writes=[pbuf[b]])
                        k.op("act", lambda e: e.copy(mtok[:, 0 if mi == 2 else 1, cb * WC:(cb + 1) * WC], psum[0:NR, b, 0:WC]),
                             reads=[pbuf[b]], writes=[mtokb])
            mtokd_b = k.buf("mtokd")
            k.dma("sp", mtok_d, mtok[:], mtokb, reads=[mtokb], writes=[mtokd_b])
            for (S, slot, goff) in ((S1T, 1, G1), (S2T, 3, G2)):
                k.op("dve", lambda e: e.tensor_scalar(S[:], modT[:, slot, :, :], 1.0, None, ALU.add), reads=[modb], writes=[modb])
                k.op("dve", lambda e: e.tensor_tensor(S[:], S[:], gT[:, goff:goff + NK].unsqueeze(2).to_broadcast([128, NK, NM]), ALU.mult),
                     reads=[modb, c2], writes=[modb])
            k.barrier()
        B1T = modT[:, 0, :, :]
        B2T = modT[:, 2, :, :]

        def norm_alloc(stk):
            xts_ = [sb([128, D], F32, f"xt{i}", stk) for i in range(2)]
            xtb_ = [k.buf(f"xt{i}") for i in range(2)]
            return (xts_, xtb_, sb([128, D], BF16, "junk", stk), k.buf("junk"), sb([128, 8], F32, "stat", stk), k.buf("stat"),
                    sb([128, 512], F32, "mtmp", stk), k.buf("mtmp"))
        sB = ExitStack()
        xts, xtb, junk, junkb, stat, statb, mtmp, mtmpb = norm_alloc(sB)

        def rms_rstd(xt, xb):
            k.op("act", lambda e: e.activation(junk[:], xt[:], AF.Square, accum_out=stat[:, 0:1]), reads=[xb], writes=[junkb, statb])
            k.op("dve", lambda e: e.tensor_scalar(stat[:, 1:2], stat[:, 0:1], 1.0 / D, EPS, ALU.mult, ALU.add), reads=[statb], writes=[statb])
            k.op("act", lambda e: e.activation(stat[:, 3:4], stat[:, 1:2], AF.Sqrt), reads=[statb], writes=[statb])
            k.op("dve", lambda e: e.reciprocal(stat[:, 2:3], stat[:, 3:4]), reads=[statb], writes=[statb])

        def norm_mod_T(xt, xb, S, Bm, sample, dst_fn, dst_bufs, f32_dst=None):
            rms_rstd(xt, xb)
            k.op("dve", lambda e: e.tensor_scalar(xt[:], xt[:], stat[:, 2:3], 0.0, ALU.mult, ALU.add), reads=[xb, statb], writes=[xb])
            for q4 in range(4):
                b = bank()

                def tr(e):
                    ins = None
                    for j in range(4):
                        kk = q4 * 4 + j
                        ins = e.transpose(psum[:, b, j * 128:(j + 1) * 128], xt[:, kk * 128:(kk + 1) * 128], idf[:])
                    return ins
                k.op("pe", tr, reads=[xb, cst], writes=[pbuf[b]])
                pv = psum[:, b, :].rearrange("p (c t) -> p c t", c=4)
                tv = mtmp[:].rearrange("p (c t) -> p c t", c=4)
                if not sample:
                    sbc = S[:, q4 * 4:q4 * 4 + 4, 0:1].to_broadcast([128, 4, 128])
                    bbc = Bm[:, q4 * 4:q4 * 4 + 4, 0:1].to_broadcast([128, 4, 128])
                    pvv, tvv = pv, tv
                    dst = dst_fn(q4 * 4)
                else:
                    sbc = S[:, q4 * 4:q4 * 4 + 4, 1:17].unsqueeze(3).to_broadcast([128, 4, NB, 8])
                    bbc = Bm[:, q4 * 4:q4 * 4 + 4, 1:17].unsqueeze(3).to_broadcast([128, 4, NB, 8])
                    pvv = psum[:, b, :].rearrange("p (c b l) -> p c b l", c=4, b=NB)
                    tvv = mtmp[:].rearrange("p (c b l) -> p c b l", c=4, b=NB)
                    dst = dst_fn(q4 * 4).rearrange("p c (b l) -> p c b l", b=NB)
                k.op("dve", lambda e: e.tensor_tensor(tvv, pvv, sbc, ALU.mult), reads=[pbuf[b], modb], writes=[mtmpb])
                k.op("dve", lambda e: e.tensor_tensor(dst, tvv, bbc, ALU.add), reads=[mtmpb, modb], writes=dst_bufs)
                if f32_dst is not None:
                    fd, fb = f32_dst
                    fdv = fd[:, q4 * 4:q4 * 4 + 4, :]
                    if sample:
                        fdv = fdv.rearrange("p c (b l) -> p c b l", b=NB)
                    k.op("dve", lambda e: e.tensor_tensor(fdv, tvv, bbc, ALU.add), reads=[mtmpb, modb], writes=[fb])

        for i in range(10):
            xt, xb = xts[i % 2], xtb[i % 2]
            k.dma("sp", xt[:], xin[i * 128:(i + 1) * 128, :], xb, writes=[xb])
            norm_mod_T(xt, xb, S1T, B1T, i == 9, lambda kk0, i=i: hT[:, kk0:kk0 + 4, i * 128:(i + 1) * 128], [hb[i]])
        k.barrier()
        sB.close()

        def hgrp_bufs(t0, n):
            return [hb[i] for i in range(t0 // 128, (t0 + n + 127) // 128)]

        def proj_fm(t, bf, j, t0, n):
            b = bank()
            pairs = [(t[:, kk, j * 128:(j + 1) * 128], hT[:, kk, t0:t0 + n]) for kk in range(NK)]
            pairs.append((t[0:1, NK, j * 128:(j + 1) * 128], ones_b[0:1, 0:n]))
            k.op("pe", mm(b, psum[:, b, 0:n], pairs), reads=[bf, c2] + hgrp_bufs(t0, n), writes=[pbuf[b]])
            return b

        with ExitStack() as s1:
            kvo = sb([128, 4, 256], F32, "kvo", s1)
            kvob = k.buf("kvo")
            for wi, c0 in enumerate((C_K, C_V)):
                tkv, bkv = wload(w_in, c0, b_in)
                for ti, i in enumerate((8, 9)):
                    b = bank()
                    pairs = [(hT[:, kk, i * 128:(i + 1) * 128], tkv[:, kk, :]) for kk in range(NK)]
                    pairs.append((ones_b[0:1, 0:128], tkv[0:1, NK, :]))
                    k.op("pe", mm(b, psum[:, b, 0:256], pairs), reads=[bkv, c2, hb[i]], writes=[pbuf[b]])
                    k.op("act", lambda e: e.copy(kvo[:, wi * 2 + ti, :], psum[:, b, 0:256]), reads=[pbuf[b]], writes=[kvob])
            k.dma("sp", klast, kvo[:, 0, :], kvob, reads=[kvob], store=True)
            k.dma("sp", vlast, kvo[:, 2, :], kvob, reads=[kvob], store=True)
            for bi in range(NB):
                k.dma("sp", ks_out[bi, 120:128, :], kvo[bi * 8:(bi + 1) * 8, 1, :], kvob, reads=[kvob], store=True)
                k.dma("sp", vs_out[bi, 120:128, :], kvo[bi * 8:(bi + 1) * 8, 3, :], kvob, reads=[kvob], store=True)
            k.dma("sp", ks_out[:, 0:120, :], cache_k[:, 8:128, :], kvob, store=True)
            k.dma("sp", vs_out[:, 0:120, :], cache_v[:, 8:128, :], kvob, store=True)
            k.barrier()

        mergedT = sb([128, NK, NOWN], BF16, "mergedT", mix)
        mgb = [k.buf(f"mg{c}") for c in range(NK)]
        cv = ExitStack()
        zT = sb([128, NK, NOWN], BF16, "zT", cv)
        zb = [k.buf(f"z{c}") for c in range(NK)]
        with ExitStack() as s1:
            uT = sb([128, NT], F32, "uT", s1)
            ub = k.buf("uT")
            sg = sb([128, 512], F32, "sg", s1)
            sgb = k.buf("sg")
            uTb = sb([128, NT], BF16, "uTb", s1)
            uTbb = k.buf("uTb")
            dg = sb([128, 31, 128], BF16, "dg", s1)
            dgb = k.buf("dg")
            uS = sb([128, 2, 128], F32, "uS", s1)
            uSb = [k.buf("uS0"), k.buf("uS1")]
            uL = sb([30, 2, 128], F32, "uL", s1)
            uLb = [k.buf("uL0"), k.buf("uL1")]
            stt = sb([120, 4, 128], F32, "stt", s1)
            sttb = k.buf("stt")
            upS = sb([128, NB, 38], F32, "upS", s1)
            upb = k.buf("upS")
            accS = sb([128, NB, 8], F32, "accS", s1)
            accSb = k.buf("accS")
            for cb in range(D // WC):
                ta, bfa = wload(w_in, C_UA + cb * WC, b_in)
                tb_, bfb = wload(w_in, C_UB + cb * WC, b_in)
                for j in range(WC // 128):
                    ch = cb * (WC // 128) + j
                    k.dma("sp", stt[:], state_conv.rearrange("(q b) r d -> (b r) q d", q=4)[:, :, ch * 128:(ch + 1) * 128], sttb, writes=[sttb])
                    for (t0, n) in GROUPS:
                        ba = proj_fm(ta, bfa, j, t0, n)
                        bb = proj_fm(tb_, bfb, j, t0, n)
                        k.op("act", lambda e: e.activation(sg[:, 0:n], psum[:, bb, 0:n], AF.Sigmoid), reads=[pbuf[bb]], writes=[sgb])
                        k.op("dve", lambda e: e.tensor_tensor(uT[:, t0:t0 + n], psum[:, ba, 0:n], sg[:, 0:n], ALU.mult),
                             reads=[pbuf[ba], sgb], writes=[ub])
                    k.op("dve", lambda e: e.tensor_scalar(uT[:, 0:TH], uT[:, 0:TH], hv[:, 0:1], 0.0, ALU.mult, ALU.add), reads=[ub, cst], writes=[ub])
                    b = bank()

                    def tr2(e):
                        e.transpose(psum[:, b, 0:128], uT[:, TH + TP:NT], idf[:])
                        return e.transpose(psum[0:30, b, 128:256], uT[:, TH + TP - 30:TH + TP], idf[:])
                    k.op("pe", tr2, reads=[ub, cst], writes=[pbuf[b]])
                    r_ = ch % 2
                    k.op("act", lambda e: e.copy(uS[:, r_, :], psum[:, b, 0:128]), reads=[pbuf[b]], writes=[uSb[r_]])
                    k.op("act", lambda e: e.copy(uL[:, r_, :], psum[0:30, b, 128:256]), reads=[pbuf[b]], writes=[uLb[r_]])
                    k.dma("sp", convlast[:, ch * 128:(ch + 1) * 128], uL[:, r_, :], uLb[r_], reads=[uLb[r_]], store=True)
                    for bi in range(NB):
                        k.dma("sp", convs_out[bi, 22:30, ch * 128:(ch + 1) * 128], uS[bi * 8:(bi + 1) * 8, r_, :], uSb[r_], reads=[uSb[r_]], store=True)
                    b = bank()

                    def tr3(e):
                        ins = None
                        for q in range(4):
                            ins = e.transpose(psum[:, b, q * 120:(q + 1) * 120], stt[:, q, :], idf[0:120, 0:120])
                        return ins
                    k.op("pe", tr3, reads=[sttb, cst], writes=[pbuf[b]])
                    k.op("act", lambda e: e.copy(upS[:, :, 0:30], psum[:, b, 0:480].rearrange("p (b r) -> p b r", r=30)), reads=[pbuf[b]], writes=[upb])
                    k.op("act", lambda e: e.copy(upS[:, :, 30:38], uT[:, TH + TP:NT].rearrange("p (b l) -> p b l", l=8)), reads=[ub], writes=[upb])
                    o0 = TH - 30
                    k.op("act", lambda e: e.copy(uTb[:, 0:TH + TP], uT[:, 0:TH + TP]), reads=[ub], writes=[uTbb])
                    k.op("dve", lambda e: e.tensor_tensor(dg[:], idb[:].unsqueeze(1).to_broadcast([128, 31, 128]),
                                                           dwT[:, ch, :].unsqueeze(2).to_broadcast([128, 31, 128]), ALU.mult), reads=[c2], writes=[dgb])
                    for t0c in (0, 512):
                        b = bank()
                        pairs = [(dg[:, jj, :], uTb[:, o0 + jj + t0c:o0 + jj + t0c + 512]) for jj in range(31)]
                        k.op("pe", mm(b, psum[:, b, 0:512], pairs), reads=[dgb, uTbb], writes=[pbuf[b]])
                        k.op("act", lambda e: e.activation(zT[:, ch, t0c:t0c + 512], psum[:, b, 0:512], AF.Identity, bias=gT[:, DWB + ch:DWB + ch + 1]),
                             reads=[pbuf[b], c2], writes=[zb[ch]])
                    k.op("dve", lambda e: e.tensor_scalar(accS[:], upS[:, :, 0:8], dwT[:, ch, 0:1], gT[:, DWB + ch:DWB + ch + 1], ALU.mult, ALU.add),
                         reads=[upb, c2], writes=[accSb])
                    for jj in range(1, 31):
                        last = jj == 30
                        dst = zT[:, ch, TP:NOWN].rearrange("p (b l) -> p b l", l=8) if last else accS[:]
                        k.op("dve", lambda e: e.scalar_tensor_tensor(dst, upS[:, :, jj:jj + 8], dwT[:, ch, jj:jj + 1], accS[:], ALU.mult, ALU.add),
                             reads=[upb, accSb, c2], writes=[zb[ch]] if last else [accSb])
            k.dma("sp", convs_out[:, 0:22, :], state_conv[:, 8:30, :], uSb[0], store=True)
            k.barrier()
        dump("zT", zT[:], zb)

        with ExitStack() as s1:
            lnA = sb([128, NOWN], F32, "lnA", s1)
            lnB = sb([128, NOWN], F32, "lnB", s1)
            lnb_ = k.buf("ln")
            zsq = sb([128, 512], BF16, "zsq", s1)
            zsqb = k.buf("zsq")
            lt = sb([128, 512], F32, "lt", s1)
            ltb = k.buf("lt")
            for (t0, n) in [(0, 512), (512, 512), (1024, 128)]:
                b1 = bank()
                b2 = bank()
                k.op("pe", mm(b1, psum[:, b1, 0:n], [(ones_b[:, 0:128], zT[:, c, t0:t0 + n]) for c in range(NK)]), reads=zb + [c2], writes=[pbuf[b1]])
                for c in range(NK):
                    k.op("act", lambda e: e.activation(zsq[:, 0:n], zT[:, c, t0:t0 + n], AF.Square), reads=[zb[c]], writes=[zsqb])
                    k.op("pe", lambda e: e.matmul(psum[:, b2, 0:n], ones_b[:, 0:128], zsq[:, 0:n], start=(c == 0), stop=(c == NK - 1)),
                         reads=[zsqb, c2], writes=[pbuf[b2]])
                A = lnA[:, t0:t0 + n]
                Bv = lnB[:, t0:t0 + n]
                k.op("dve", lambda e: e.tensor_scalar(Bv, psum[:, b1, 0:n], 1.0 / D, None, ALU.mult), reads=[pbuf[b1]], writes=[lnb_])
                k.op("dve", lambda e: e.tensor_tensor(lt[:, 0:n], Bv, Bv, ALU.mult), reads=[lnb_], writes=[ltb])
                k.op("dve", lambda e: e.scalar_tensor_tensor(A, psum[:, b2, 0:n], 1.0 / D, lt[:, 0:n], ALU.mult, ALU.subtract), reads=[pbuf[b2], ltb], writes=[lnb_])
                k.op("dve", lambda e: e.tensor_scalar(A, A, EPS, None, ALU.add), reads=[lnb_], writes=[lnb_])
                k.op("act", lambda e: e.activation(A, A, AF.Sqrt), reads=[lnb_], writes=[lnb_])
                k.op("dve", lambda e: e.reciprocal(A, A), reads=[lnb_], writes=[lnb_])
                k.op("dve", lambda e: e.scalar_tensor_tensor(Bv, Bv, -1.0, A, ALU.mult, ALU.mult), reads=[lnb_], writes=[lnb_])
            for c in range(NK):
                for (t0, n) in [(0, 512), (512, 512), (1024, 128)]:
                    k.op("dve", lambda e: e.tensor_tensor(lt[:, 0:n], zT[:, c, t0:t0 + n], lnA[:, t0:t0 + n], ALU.mult), reads=[zb[c], lnb_], writes=[ltb])
                    k.op("dve", lambda e: e.tensor_tensor(lt[:, 0:n], lt[:, 0:n], lnB[:, t0:t0 + n], ALU.add), reads=[ltb, lnb_], writes=[ltb])
                    k.op("act", lambda e: e.activation(zT[:, c, t0:t0 + n], lt[:, 0:n], AF.Silu, bias=gT[:, LNB + c:LNB + c + 1], scale=gT[:, LNG + c:LNG + c + 1]),
                         reads=[ltb, c2], writes=[zb[c]])
            for cb in range(D // WC):
                tw, bw = wload(w_conv_out, cb * WC, b_conv_out)
                tg, bg = wload(w_in, C_GC + cb * WC, b_in)
                for j in range(WC // 128):
                    ch = cb * (WC // 128) + j
                    for (t0, n) in OGROUPS:
                        z0 = t0 - TH
                        b1 = bank()
                        pairs = [(tw[:, kk, j * 128:(j + 1) * 128], zT[:, kk, z0:z0 + n]) for kk in range(NK)]
                        pairs.append((tw[0:1, NK, j * 128:(j + 1) * 128], ones_b[0:1, 0:n]))
                        k.op("pe", mm(b1, psum[:, b1, 0:n], pairs), reads=[bw, c2] + zb, writes=[pbuf[b1]])
                        b2 = proj_fm(tg, bg, j, t0, n)
                        k.op("act", lambda e: e.activation(lt[:, 0:n], psum[:, b2, 0:n], AF.Sigmoid), reads=[pbuf[b2]], writes=[ltb])
                        k.op("dve", lambda e: e.tensor_tensor(mergedT[:, ch, z0:z0 + n], psum[:, b1, 0:n], lt[:, 0:n], ALU.mult),
                             reads=[pbuf[b1], ltb], writes=[mgb[ch]])
            k.barrier()
        cv.close()
        dump("mg1", mergedT[:], mgb)

        at = ExitStack()
        attnT = sb([128, NK, NOWN], BF16, "attnT", at)
        atb = [k.buf(f"at{c}") for c in range(NK)]
        with ExitStack() as s1:
            qT = sb([128, 4, NOWN], BF16, "qT", s1)
            qb = k.buf("qT")
            kTp = sb([128, 2, NT], BF16, "kTp", s1)
            kTb = k.buf("kTp")
            vP = sb([128, 10, 2, 128], BF16, "vP", s1)
            vPb = k.buf("vP")
            wkp = sb([128, NK + 1, 2, 128], BF16, "wkp", s1)
            wkb = k.buf("wkp")
            wv = sb([128, NK + 1, 64], BF16, "wv", s1)
            wvb = k.buf("wv")
            ckf = sb([128, NB, 64], F32, "ckf", s1)
            ckb = k.buf("ckf")
            cvf = sb([128, NB, 64], F32, "cvf", s1)
            cvb = k.buf("cvf")
            qS = sb([128, NB, 32], BF16, "qS", s1)
            qSb = k.buf("qS")
            ck2 = [sb([128, 128], F32, f"ck2{i}", s1) for i in range(2)]
            ck2b = [k.buf(f"ck2{i}") for i in range(2)]
            kcp = [sb([128, 2, 128], BF16, f"kcp{i}", s1) for i in range(2)]
            kcb = [k.buf(f"kcp{i}") for i in range(2)]
            vcp = [sb([128, 2, 128], BF16, f"vcp{i}", s1) for i in range(2)]
            vcb = [k.buf(f"vcp{i}") for i in range(2)]
            pbf = [sb([128, 4, 256], BF16, f"pbf{i}", s1) for i in range(2)]
            pbfb = [k.buf(f"pbf{i}") for i in range(2)]
            pT = [sb([128, 8, 128], BF16, f"pT{i}", s1) for i in range(2)]
            pTb = [k.buf(f"pT{i}") for i in range(2)]
            sm = [sb([128, 32], F32, f"sm{i}", s1) for i in range(2)]
            smb = [k.buf(f"sm{i}") for i in range(2)]
            mkc = sb([32, 128], F32, "mkc", s1)
            mkw = sb([32, 248], F32, "mkw", s1)
            sks = sb([32, 8], F32, "sks", s1)
            mksb = k.buf("mks")
            k.dma("sp", mkc[:], maskc_in, mksb, writes=[mksb])
            k.dma("sp", mkw[:], maskw_in, mksb, writes=[mksb], join=True)
            k.dma("sp", sks[:], sinks_s_in, mksb, writes=[mksb], join=True)
            k.op("dve", lambda e: e.memset(kTp[:], 0.0), writes=[kTb])
            k.op("dve", lambda e: e.memset(vP[:], 0.0), writes=[vPb])
            k.op("dve", lambda e: e.memset(wkp[:], 0.0), writes=[wkb])
            for p_ in range(2):
                k.op("dve", lambda e: e.memset(kcp[p_][:], 0.0), writes=[kcb[p_]])
                k.op("dve", lambda e: e.memset(vcp[p_][:], 0.0), writes=[vcb[p_]])
            pT_ps = psum[:].bitcast(BF16)

            def run_interleaved(gens):
                active = list(gens)
                while active:
                    for g_ in list(active):
                        try:
                            next(g_)
                        except StopIteration:
                            active.remove(g_)

            def softmax_block(np_, bsc, nh, maskap, sink_ap, par):
                S_ = psum[0:np_, bsc:bsc + 2, :].rearrange("p a (h s) -> p (a h) s", s=256)[:, 0:nh, :]
                pb2 = [pbuf[bsc], pbuf[bsc + 1]]
                smp, smpb = sm[par], smb[par]
                for (c0_, c1_, map_) in maskap:
                    k.op("dve", lambda e: e.tensor_tensor(S_[:, :, c0_:c1_], S_[:, :, c0_:c1_], map_, ALU.add), reads=pb2 + [cst, mksb], writes=pb2)
                    yield
                mx = smp[0:np_, 0:nh]
                k.op("dve", lambda e: e.reduce_max(mx, S_, AX.X), reads=pb2, writes=[smpb])
                yield
                k.op("dve", lambda e: e.tensor_tensor(mx, mx, sink_ap, ALU.max), reads=[smpb, cst, mksb], writes=[smpb])
                yield
                es = smp[0:np_, 8:8 + nh]
                k.op("dve", lambda e: e.tensor_tensor(es, sink_ap, mx, ALU.subtract), reads=[smpb, cst, mksb], writes=[smpb])
                yield
                k.op("dve", lambda e: e.tensor_tensor(S_, S_, mx.unsqueeze(2).to_broadcast([np_, nh, 256]), ALU.subtract), reads=pb2 + [smpb], writes=pb2)
                yield
                k.op("act", lambda e: e.activation(es, es, AF.Exp), reads=[smpb], writes=[smpb])
                yield
                k.op("act", lambda e: e.activation(S_, S_, AF.Exp), reads=pb2, writes=pb2)
                yield
                den = smp[0:np_, 16:16 + nh]
                k.op("dve", lambda e: e.reduce_sum(den, S_, AX.X), reads=pb2, writes=[smpb])
                yield
                k.op("dve", lambda e: e.tensor_tensor(den, den, es, ALU.add), reads=[smpb], writes=[smpb])
                yield
                k.op("dve", lambda e: e.reciprocal(den, den), reads=[smpb], writes=[smpb])
                yield
                k.op("dve", lambda e: e.tensor_tensor(pbf[par][0:np_, 0:nh, :], S_, den.unsqueeze(2).to_broadcast([np_, nh, 256]), ALU.mult),
                     reads=pb2 + [smpb], writes=[pbfb[par]])
                yield

            def prompt_step(g, n_, cp, par):
                bsc, bt, bo = 2 * par, 4 + par, 6 + par
                for hh in range(4):
                    cl = cp * 2 + hh // 2
                    eo = hh % 2
                    k.op("pe", lambda e: e.matmul(psum[:, bsc + hh // 2, (hh % 2) * 256:(hh % 2) * 256 + 256], qT[:, cl, n_ * 128:(n_ + 1) * 128],
                                                   kTp[:, eo, n_ * 128:n_ * 128 + 256], start=True, stop=True),
                         reads=[qb, kTb], writes=[pbuf[bsc + hh // 2]])
                yield
                mk = [(0, 256, (mk0 if n_ == 0 else mk1)[:].unsqueeze(1).to_broadcast([128, 4, 256]))]
                h0 = g * 8 + cp * 4
                yield from softmax_block(128, bsc, 4, mk, sinks[:, h0:h0 + 4], par)

                def trp(e):
                    ins = None
                    for hh in range(4):
                        for kb in range(2):
                            ins = e.transpose(pT_ps[:, bt, (hh * 2 + kb) * 128:(hh * 2 + kb + 1) * 128], pbf[par][:, hh, kb * 128:(kb + 1) * 128], idb[:])
                    return ins
                k.op("pe", trp, reads=[pbfb[par], c2], writes=[pbuf[bt]])
                yield
                k.op("act", lambda e: e.copy(pT[par][:].rearrange("p a t -> p (a t)"), pT_ps[:, bt, :]), reads=[pbuf[bt]], writes=[pTb[par]])
                yield
                for c2_ in range(2):
                    def pv(e):
                        ins = None
                        for eo in range(2):
                            for kb in range(2):
                                hh = c2_ * 2 + eo
                                ins = e.matmul(psum[:, bo, c2_ * 128:(c2_ + 1) * 128], vP[:, n_ + kb, eo, :], pT[par][:, hh * 2 + kb, :],
                                               start=(eo == 0 and kb == 0), stop=(eo == 1 and kb == 1))
                        return ins
                    k.op("pe", pv, reads=[vPb, pTb[par]], writes=[pbuf[bo]])
                yield
                ch0 = g * 4 + cp * 2
                k.op("act", lambda e: e.copy(attnT[:, ch0:ch0 + 2, n_ * 128:(n_ + 1) * 128], psum[:, bo, 0:256].rearrange("p (c t) -> p c t", c=2)),
                     reads=[pbuf[bo]], writes=[atb[ch0], atb[ch0 + 1]])
                yield

            def sample_step(g, bi, par):
                bsc, bt, bo = 2 * par, 4 + par, 6 + par
                ck2_, kcp_, vcp_ = ck2[par], kcp[par], vcp[par]
                k.op("act", lambda e: e.copy(ck2_[:, 0:64], ckf[:, bi, :]), reads=[ckb], writes=[ck2b[par]])
                k.op("act", lambda e: e.copy(ck2_[:, 64:128], ckf[:, bi, :]), reads=[ckb], writes=[ck2b[par]])
                yield
                k.op("dve", lambda e: e.tensor_copy(vcp_[:, 0, 0:64], cvf[:, bi, :]), reads=[cvb], writes=[vcb[par]])
                k.op("dve", lambda e: e.tensor_copy(vcp_[:, 1, 64:128], cvf[:, bi, :]), reads=[cvb], writes=[vcb[par]])
                yield
                k.op("pe", lambda e: e.transpose(psum[:, bo, 0:128], ck2_[:], idf[:]), reads=[ck2b[par], cst], writes=[pbuf[bo]])
                yield
                k.op("act", lambda e: e.copy(kcp_[0:64, 0, :], psum[0:64, bo, 0:128]), reads=[pbuf[bo]], writes=[kcb[par]])
                k.op("act", lambda e: e.copy(kcp_[64:128, 1, :], psum[64:128, bo, 0:128]), reads=[pbuf[bo]], writes=[kcb[par]])
                yield
                s0_ = TP + bi * 8
                for eo in range(2):
                    lq = qS[:, bi, :]
                    k.op("pe", lambda e: e.matmul(psum[0:32, bsc, eo * 256:eo * 256 + 128], lq, kcp_[:, eo, :], start=True, stop=True),
                         reads=[qSb, kcb[par]], writes=[pbuf[bsc]])
                    k.op("pe", lambda e: e.matmul(psum[0:32, bsc, eo * 256 + 128:eo * 256 + 256], lq, kTp[:, eo, TH + TP:NT], start=True, stop=True),
                         reads=[qSb, kTb], writes=[pbuf[bsc]])
                yield
                yield from softmax_block(32, bsc, 2, [(0, 128, mkc[:].unsqueeze(1).to_broadcast([32, 2, 128])),
                                                      (128, 256, mkw[:, 120 - 8 * bi:248 - 8 * bi].unsqueeze(1).to_broadcast([32, 2, 128]))], sks[:, g * 2:g * 2 + 2], par)

                def trs(e):
                    ins = None
                    for eo in range(2):
                        for kb in range(2):
                            ins = e.transpose(pT_ps[:, bt, (eo * 2 + kb) * 128:(eo * 2 + kb) * 128 + 32], pbf[par][0:32, eo, kb * 128:(kb + 1) * 128], idb[0:32, 0:32])
                    return ins
                k.op("pe", trs, reads=[pbfb[par], c2], writes=[pbuf[bt]])
                yield
                k.op("act", lambda e: e.copy(pT[par][:, 0:4, 0:32], pT_ps[:, bt, 0:512].rearrange("p (a t) -> p a t", t=128)[:, :, 0:32]), reads=[pbuf[bt]], writes=[pTb[par]])
                yield
                for cl in range(4):
                    def pvs(e):
                        ins = None
                        for eo in range(2):
                            ins = e.matmul(psum[:, bo, cl * 8:(cl + 1) * 8], vcp_[:, eo, :], pT[par][:, eo * 2 + 0, cl * 8:(cl + 1) * 8], start=(eo == 0), stop=False)
                            ins = e.matmul(psum[:, bo, cl * 8:(cl + 1) * 8], vP[:, 9, eo, :], pT[par][:, eo * 2 + 1, cl * 8:(cl + 1) * 8], start=False, stop=(eo == 1))
                        return ins
                    k.op("pe", pvs, reads=[vcb[par], vPb, pTb[par]], writes=[pbuf[bo]])
                yield
                ch0 = g * 4
                k.op("act", lambda e: e.copy(attnT[:, ch0:ch0 + 4, s0_:s0_ + 8], psum[:, bo, 0:32].rearrange("p (c l) -> p c l", l=8)),
                     reads=[pbuf[bo]], writes=[atb[ch0 + i_] for i_ in range(4)])
                yield

            for g in range(4):
                for half in range(2):
                    k.dma("pool", wkp[:, 0:NK, half, half * 64:(half + 1) * 64],
                          w_in.rearrange("(k p) c -> p k c", p=128)[:, :, C_K + g * 64:C_K + (g + 1) * 64], wkb, writes=[wkb], join=(half == 1))
                    k.dma("pool", wkp[0:1, NK, half, half * 64:(half + 1) * 64], b_in[0:1, C_K + g * 64:C_K + (g + 1) * 64], wkb, writes=[wkb], join=True)
                k.dma("pool", wv[:, 0:NK, :], w_in.rearrange("(k p) c -> p k c", p=128)[:, :, C_V + g * 64:C_V + (g + 1) * 64], wvb, writes=[wvb])
                k.dma("pool", wv[0:1, NK, :], b_in[0:1, C_V + g * 64:C_V + (g + 1) * 64], wvb, writes=[wvb], join=True)
                for half in range(2):
                    for (t0, n) in GROUPS:
                        b = bank()
                        pairs = [(wkp[:, kk, half, :], hT[:, kk, t0:t0 + n]) for kk in range(NK)]
                        pairs.append((wkp[0:1, NK, half, :], ones_b[0:1, 0:n]))
                        k.op("pe", mm(b, psum[:, b, 0:n], pairs), reads=[wkb, c2] + hgrp_bufs(t0, n), writes=[pbuf[b]])
                        k.op("act", lambda e: e.copy(kTp[:, half, t0:t0 + n], psum[:, b, 0:n]), reads=[pbuf[b]], writes=[kTb])
                for i in range(10):
                    b = bank()
                    pairs = [(hT[:, kk, i * 128:(i + 1) * 128], wv[:, kk, :]) for kk in range(NK)]
                    pairs.append((ones_b[0:1, 0:128], wv[0:1, NK, :]))
                    k.op("pe", mm(b, psum[:, b, 0:64], pairs), reads=[wvb, c2, hb[i]], writes=[pbuf[b]])
                    k.op("act", lambda e: e.copy(vP[:, i, 0, 0:64], psum[:, b, 0:64]), reads=[pbuf[b]], writes=[vPb])
                    k.op("act", lambda e: e.copy(vP[:, i, 1, 64:128], psum[:, b, 0:64]), reads=[pbuf[b]], writes=[vPb])
                for cc in range(2):
                    tq, bq = wload(w_in, C_Q + g * 512 + cc * WC, b_in)
                    for j in range(2):
                        for (t0, n) in OGROUPS:
                            b = proj_fm(tq, bq, j, t0, n)
                            k.op("act", lambda e: e.activation(qT[:, cc * 2 + j, t0 - TH:t0 - TH + n], psum[:, b, 0:n], AF.Copy, scale=0.125),
                                 reads=[pbuf[b]], writes=[qb])
                steps = [(n_, cp) for n_ in range(8) for cp in range(2)]
                for i_ in range(0, len(steps), 2):
                    run_interleaved([prompt_step(g, steps[i_ + p_][0], steps[i_ + p_][1], p_) for p_ in range(2)])
                k.dma("sp", ckf[:], cache_k.rearrange("b s d -> s b d")[:, :, g * 64:(g + 1) * 64], ckb, writes=[ckb])
                k.dma("sp", cvf[:], cache_v.rearrange("b s d -> s b d")[:, :, g * 64:(g + 1) * 64], cvb, writes=[cvb])
                k.op("dve", lambda e: e.tensor_copy(qS[:].rearrange("p b (c l) -> p b c l", c=4), qT[:, :, TP:NOWN].rearrange("p c (b l) -> p b c l", l=8)),
                     reads=[qb], writes=[qSb])
                for bi in range(0, NB, 2):
                    run_interleaved([sample_step(g, bi + p_, p_) for p_ in range(2)])
            k.barrier()
        dump("attnT", attnT[:], atb)

        with ExitStack() as s1:
            lt = sb([128, 512], F32, "lt2", s1)
            ltb = k.buf("lt2")
            l2 = sb([128, 512], F32, "l2", s1)
            l2b = k.buf("l2")
            for cb in range(D // WC):
                tw, bw = wload(w_attn_out, cb * WC, b_attn_out)
                tg, bg = wload(w_in, C_GA + cb * WC, b_in)
                for j in range(WC // 128):
                    ch = cb * (WC // 128) + j
                    for (t0, n) in OGROUPS:
                        z0 = t0 - TH
                        b1 = bank()
                        pairs = [(tw[:, kk, j * 128:(j + 1) * 128], attnT[:, kk, z0:z0 + n]) for kk in range(NK)]
                        pairs.append((tw[0:1, NK, j * 128:(j + 1) * 128], ones_b[0:1, 0:n]))
                        k.op("pe", mm(b1, psum[:, b1, 0:n], pairs), reads=[bw, c2] + atb, writes=[pbuf[b1]])
                        b2 = proj_fm(tg, bg, j, t0, n)
                        k.op("act", lambda e: e.activation(lt[:, 0:n], psum[:, b2, 0:n], AF.Sigmoid), reads=[pbuf[b2]], writes=[ltb])
                        k.op("dve", lambda e: e.tensor_tensor(l2[:, 0:n], psum[:, b1, 0:n], lt[:, 0:n], ALU.mult), reads=[pbuf[b1], ltb], writes=[l2b])
                        k.op("dve", lambda e: e.tensor_tensor(mergedT[:, ch, z0:z0 + n], l2[:, 0:n], mergedT[:, ch, z0:z0 + n], ALU.add),
                             reads=[l2b, mgb[ch]], writes=[mgb[ch]])
            k.barrier()
        at.close()
        dump("mg2", mergedT[:], mgb)

        x1b = [k.buf(f"x1_{i}") for i in range(9)]
        h2db = k.buf("h2d")
        with ExitStack() as s1:
            mtok = sb([NR, 2, D], F32, "mtok2", s1)
            mtokb = k.buf("mtok2")
            k.dma("sp", mtok[:], mtok_d, mtokb, reads=[mtokd_b], writes=[mtokb])
            m2 = sb([128, 2, WC], F32, "m2", s1)
            m2b = k.buf("m2")
            xc = [sb([128, WC], F32, f"xc{i}", s1) for i in range(2)]
            xcb = [k.buf(f"xc{i}") for i in range(2)]
            t1 = [sb([128, WC], F32, f"t1{i}", s1) for i in range(2)]
            t1b = [k.buf(f"t1{i}") for i in range(2)]
            it = 0
            for cb in range(D // WC):
                tw, bw = wload(w_out, cb * WC, None)
                cols = slice(cb * WC, (cb + 1) * WC)
                b = bank()

                def selmm(e):
                    e.matmul(psum[:, b, 0:WC], sel[:, 0:128], mtok[:, 0, cols], start=True, stop=True)
                    return e.matmul(psum[:, b, WC:2 * WC], sel[:, 128:256], mtok[:, 0, cols], start=True, stop=True)
                k.op("pe", selmm, reads=[cst, mtokb], writes=[pbuf[b]])
                k.op("act", lambda e: e.copy(m2[:].rearrange("p a c -> p (a c)"), psum[:, b, 0:2 * WC]), reads=[pbuf[b]], writes=[m2b])
                for i in range(9):
                    r = it % 2
                    it += 1
                    b = bank()
                    pairs = [(mergedT[:, kk, i * 128:(i + 1) * 128], tw[:, kk, :]) for kk in range(NK)]
                    k.op("pe", mm(b, psum[:, b, 0:WC], pairs), reads=[bw] + mgb, writes=[pbuf[b]])
                    k.dma("sp", xc[r][:], xin[TH + i * 128:TH + (i + 1) * 128, cols], xcb[r], writes=[xcb[r]])
                    k.op("dve", lambda e: e.tensor_tensor(t1[r][:], psum[:, b, 0:WC], m2[:, 1 if i == 8 else 0, :], ALU.mult), reads=[pbuf[b], m2b], writes=[t1b[r]])
                    k.op("dve", lambda e: e.tensor_tensor(t1[r][:], t1[r][:], xc[r][:], ALU.add), reads=[t1b[r], xcb[r]], writes=[t1b[r]])
                    k.dma("sp", x1_d[i * 128:(i + 1) * 128, cols], t1[r][:], t1b[r], reads=[t1b[r]], writes=[x1b[i]], join=True)
            m5 = sb([128, D], F32, "m5", s1)
            m5b = k.buf("m5")
            for j2 in range(2):
                for cg in range(4):
                    b = bank()
                    k.op("pe", lambda e: e.matmul(psum[:, b, 0:512], sel[:, j2 * 128:(j2 + 1) * 128], mtok[:, 1, cg * 512:(cg + 1) * 512], start=True, stop=True),
                         reads=[cst, mtokb], writes=[pbuf[b]])
                    k.op("act", lambda e: e.copy(m5[:, cg * 512:(cg + 1) * 512], psum[:, b, 0:512]), reads=[pbuf[b]], writes=[m5b])
                k.dma("sp", m5_d[j2], m5[:], m5b, reads=[m5b], writes=[x1b[0]], join=True)
            k.barrier()

        with ExitStack() as s1:
            wr = sb([128, NK, NE], F32, "wr", s1)
            br = sb([1, NE], F32, "br", s1)
            wrb = k.buf("wr")
            k.dma("sp", wr[:], w_router.rearrange("(k p) e -> p k e", p=128), wrb, writes=[wrb])
            k.dma("sp", br[:], b_router, wrb, writes=[wrb], join=True)
            h2f = sb([128, NK, 128], F32, "h2f", s1)
            h2fb = k.buf("h2f")
            rt = sb([128, 4, NE], F32, "rt", s1)
            rtb = k.buf("rt")
            xts, xtb, junk, junkb, stat, statb, mtmp, mtmpb = norm_alloc(s1)
            pT2 = psum[:].bitcast(BF16)
            h2tk = [sb([128, D], BF16, f"h2tk{i}", s1) for i in range(2)]
            h2tkb = [k.buf(f"h2tk{i}") for i in range(2)]
            for i in range(9):
                xt, xb = xts[i % 2], xtb[i % 2]
                k.dma("sp", xt[:], x1_d[i * 128:(i + 1) * 128, :], xb, reads=[x1b[i]], writes=[xb])
                norm_mod_T(xt, xb, S2T, B2T, i == 8, lambda kk0, i=i: h2T[:, kk0:kk0 + 4, i * 128:(i + 1) * 128], [h2b[i]], f32_dst=(h2f, h2fb))
                bt2 = bank2()

                def trb(e):
                    ins = None
                    for kk in range(NK):
                        ins = e.transpose(pT2[:, bt2 + kk // 8, (kk % 8) * 128:(kk % 8 + 1) * 128], h2T[:, kk, i * 128:(i + 1) * 128], idb[:])
                    return ins
                k.op("pe", trb, reads=[h2b[i], c2], writes=[pbuf[bt2], pbuf[bt2 + 1]])
                hk = h2tk[i % 2]
                k.op("act", lambda e: e.copy(hk[:].rearrange("p (a c) -> p a c", a=2), pT2[:, bt2:bt2 + 2, :]), reads=[pbuf[bt2], pbuf[bt2 + 1]], writes=[h2tkb[i % 2]])
                k.dma("sp", h2_d[i], hk[:], h2tkb[i % 2], reads=[h2tkb[i % 2]], writes=[h2db])
                b = bank()
                pairs = [(h2f[:, kk, :], wr[:, kk, :]) for kk in range(NK)]
                pairs.append((ones_f[0:1, 0:128], br[0:1, :]))
                k.op("pe", mm(b, psum[:, b, 0:NE], pairs), reads=[h2fb, wrb, c2], writes=[pbuf[b]])
                lg = rt[:, 0, :]
                k.op("act", lambda e: e.copy(lg, psum[:, b, 0:NE]), reads=[pbuf[b]], writes=[rtb])
                m8 = rt[:, 1, 0:8]
                k.op("dve", lambda e: e.max(m8, lg), reads=[rtb], writes=[rtb])
                msk = rt[:, 2, :]
                k.op("dve", lambda e: e.tensor_scalar(msk, lg, rt[:, 1, 3:4], 0.0, ALU.is_ge, ALU.add), reads=[rtb], writes=[rtb])
                k.op("dve", lambda e: e.tensor_scalar(rt[:, 1, 8:9], rt[:, 1, 0:1], -1.0, None, ALU.mult), reads=[rtb], writes=[rtb])
                ex = rt[:, 3, :]
                k.op("act", lambda e: e.activation(ex, lg, AF.Exp, bias=rt[:, 1, 8:9]), reads=[rtb], writes=[rtb])
                k.op("dve", lambda e: e.tensor_tensor(ex, ex, msk, ALU.mult), reads=[rtb], writes=[rtb])
                k.op("dve", lambda e: e.reduce_sum(rt[:, 1, 9:10], ex, AX.X), reads=[rtb], writes=[rtb])
                k.op("dve", lambda e: e.reciprocal(rt[:, 1, 10:11], rt[:, 1, 9:10]), reads=[rtb], writes=[rtb])
                k.op("dve", lambda e: e.tensor_scalar(comb[:, i, :], ex, rt[:, 1, 10:11], 0.0, ALU.mult, ALU.add), reads=[rtb], writes=[combb])
            k.barrier()
        dump("h2T", h2T[:], h2b)
        dump("comb", comb[:], [combb])
        if stage < 5:
            if "x1" in dbg_names:
                o = dout("dbg_x1", [NOWN, D])
                k.dma("sp", o, x1_d, dbgq, reads=x1b, store=True)
            k.barrier()
            mix.close()
            k.finish()
            return nc
        k.barrier()
        mix.close()

        CAP = MOE_CAP
        NJT = CAP // 128
        acc = sb([128, 9, D], F32, "acc")
        accb = [k.buf(f"acc{i}") for i in range(9)]
        with ExitStack() as s1:
            h2k = sb([128, 9, D], BF16, "h2k", s1)
            h2kb = k.buf("h2k")
            k.dma("sp", h2k[:], h2_d.rearrange("i p d -> p i d"), h2kb, reads=[h2db], writes=[h2kb])
            utf = sb([128, 128], F32, "utf", s1)
            utb_ = sb([128, 128], BF16, "utb", s1)
            iotac = sb([128, CAP], F32, "iotac", s1)
            iotap = sb([128, 9], F32, "iotap", s1)
            cmb = k.buf("cm")
            k.dma("sp", utf[:], utri_in, cmb, writes=[cmb])
            k.dma("sp", iotac[:], iotac_in[:, 0:CAP], cmb, writes=[cmb], join=True)
            k.dma("sp", iotap[:], iotap_in, cmb, writes=[cmb], join=True)
            c3 = k.buf("c3")
            k.op("dve", lambda e: e.tensor_copy(utb_[:], utf[:]), reads=[cmb], writes=[c3])
            for i in range(9):
                k.op("dve", lambda e: e.memset(acc[:, i, :], 0.0), writes=[accb[i]])
            maskf = sb([128, 9, NE], F32, "maskf", s1)
            maskb = sb([128, 9, NE], BF16, "maskb", s1)
            posm = sb([128, 9, NE], F32, "posm", s1)
            posT = sb([NE, NOWN], F32, "posT", s1)
            mkb_ = k.buf("mask")
            k.op("dve", lambda e: e.tensor_scalar(maskf[:], comb[:], 0.0, None, ALU.is_gt), reads=[combb], writes=[mkb_])
            k.op("dve", lambda e: e.tensor_copy(maskb[:], maskf[:]), reads=[mkb_], writes=[mkb_])
            posb = k.buf("pos")
            for i in range(9):
                b = bank()
                pairs = [(utb_[:], maskb[:, i, :])] + [(ones_b[:, 0:128], maskb[:, ii, :]) for ii in range(i)]
                k.op("pe", mm(b, psum[:, b, 0:NE], pairs), reads=[mkb_, c3, c2], writes=[pbuf[b]])
                k.op("dve", lambda e: e.scalar_tensor_tensor(posm[:, i, :], psum[:, b, 0:NE], 1.0, maskf[:, i, :], ALU.add, ALU.mult), reads=[pbuf[b], mkb_], writes=[posb])
                k.op("dve", lambda e: e.tensor_scalar(posm[:, i, :], posm[:, i, :], -1.0, None, ALU.add), reads=[posb], writes=[posb])
                b = bank()
                k.op("pe", lambda e: e.transpose(psum[0:NE, b, 0:128], posm[:, i, :], idf[:]), reads=[posb, cst], writes=[pbuf[b]])
                k.op("act", lambda e: e.copy(posT[:, i * 128:(i + 1) * 128], psum[0:NE, b, 0:128]), reads=[pbuf[b]], writes=[posb])
            NWIN = NOWN // CAP
            flg = sb([1, NWIN * NE], F32, "flg", s1)
            fli = sb([1, NWIN * NE], mybir.dt.int32, "fli", s1)
            flb = k.buf("flg")
            b = bank()
            k.op("pe", mm(b, psum[0:1, b, 0:NE], [(ones_b[:, 0:1], maskb[:, i, :]) for i in range(9)]), reads=[mkb_, c2], writes=[pbuf[b]])
            k.op("dve", lambda e: e.memset(flg[:], 0.0), writes=[flb])
            for w in range(1, NWIN):
                k.op("dve", lambda e: e.tensor_scalar(flg[:, w * NE:(w + 1) * NE], psum[0:1, b, 0:NE], float(w * CAP), None, ALU.is_gt), reads=[pbuf[b]], writes=[flb])
            k.op("dve", lambda e: e.tensor_copy(fli[:], flg[:]), reads=[flb], writes=[flb])
            k.dma("sp", flags_d[:, 0:NWIN * NE], fli[:], flb, reads=[flb])

            Se = [sb([128, 9, CAP], BF16, f"Se{i}", s1) for i in range(1)]
            Seb = [k.buf(f"Se{i}") for i in range(2)]
            STe = [sb([128, NJT, NOWN], BF16, f"STe{i}", s1) for i in range(1)]
            STb = [k.buf(f"STe{i}") for i in range(2)]
            XeT = [sb([128, NK, CAP], BF16, f"XeT{i}", s1) for i in range(1)]
            Xb = [k.buf(f"XeT{i}") for i in range(2)]
            actT = sb([128, NK, CAP], BF16, "actT", s1)
            actb = [k.buf(f"act{f}") for f in range(NK)]
            Ye = sb([128, NJT, D], BF16, "Ye", s1)
            Yeb = k.buf("Ye")
            pr = sb([NE, NOWN], F32, "pr", s1)
            prb = k.buf("pr")
            tA = sb([128, CAP], F32, "tA", s1)
            tB = sb([128, CAP], F32, "tB", s1)
            tC = sb([128, CAP], F32, "tC", s1)
            tAb, tBb, tCb = k.buf("tA"), k.buf("tB"), k.buf("tC")
            MG = [(0, 512), (512, 512), (1024, 128)]
            def emit_expert(ex_, w):
                r_ = 0
                S_, St_, X_ = Se[r_], STe[r_], XeT[r_]
                for i in range(9):
                    k.op("dve", lambda e: e.tensor_scalar(S_[:, i, :], iotac[:], float(w * CAP), posm[:, i, ex_:ex_ + 1], ALU.add, ALU.is_equal),
                         reads=[cmb, posb], writes=[Seb[r_]])
                k.op("dve", lambda e: e.tensor_scalar(pr[:], posT[:], idf[0:NE, ex_:ex_ + 1], 0.0, ALU.mult, ALU.add), reads=[posb, cst], writes=[prb])
                for (t0, n) in MG:
                    b = bank()
                    k.op("pe", lambda e: e.matmul(psum[:, b, 0:n], ones_f[:, 0:128], pr[:, t0:t0 + n], start=True, stop=True), reads=[prb, c2], writes=[pbuf[b]])
                    for jt in range(NJT):
                        k.op("dve", lambda e: e.tensor_scalar(St_[:, jt, t0:t0 + n], psum[:, b, 0:n], iotap[:, w * NJT + jt:w * NJT + jt + 1], 0.0, ALU.is_equal, ALU.add),
                             reads=[pbuf[b], cmb], writes=[STb[r_]])
                for kk in range(NK):
                    b = bank()
                    pairs = [(h2k[:, i, kk * 128:(kk + 1) * 128], S_[:, i, :]) for i in range(9)]
                    k.op("pe", mm(b, psum[:, b, 0:CAP], pairs), reads=[h2kb, Seb[r_]], writes=[pbuf[b]])
                    k.op("act", lambda e: e.copy(X_[:, kk, :], psum[:, b, 0:CAP]), reads=[pbuf[b]], writes=[Xb[r_]])
                for f in range(NK):
                    t1_, bf1 = wload(w_mlp1[ex_], f * WC, b_mlp1[ex_])
                    bg_ = bank()
                    bl_ = bank()
                    for (bb_, off) in ((bg_, 0), (bl_, 1)):
                        pairs = [(t1_[:, kk, off:WC:2], X_[:, kk, :]) for kk in range(NK)]
                        pairs.append((t1_[0:1, NK, off:WC:2], ones_b[0:1, 0:CAP]))
                        k.op("pe", mm(bb_, psum[:, bb_, 0:CAP], pairs), reads=[bf1, c2, Xb[r_]], writes=[pbuf[bb_]])
                    k.op("dve", lambda e: e.tensor_scalar(tA[:], psum[:, bg_, 0:CAP], 7.0, None, ALU.min), reads=[pbuf[bg_]], writes=[tAb])
                    k.op("act", lambda e: e.activation(tB[:], tA[:], AF.Sigmoid, scale=1.702), reads=[tAb], writes=[tBb])
                    k.op("dve", lambda e: e.tensor_scalar(tC[:], psum[:, bl_, 0:CAP], 7.0, -7.0, ALU.min, ALU.max), reads=[pbuf[bl_]], writes=[tCb])
                    k.op("dve", lambda e: e.scalar_tensor_tensor(tC[:], tC[:], 1.0, tA[:], ALU.add, ALU.mult), reads=[tCb, tAb], writes=[tCb])
                    k.op("dve", lambda e: e.tensor_tensor(actT[:, f, :], tC[:], tB[:], ALU.mult), reads=[tCb, tBb], writes=[actb[f]])
                for db in range(D // WC):
                    t2_, bf2 = wload(w_mlp2[ex_], db * WC, b_mlp2[ex_])
                    for jt in range(NJT):
                        b = bank()
                        pairs = [(actT[:, f, jt * 128:(jt + 1) * 128], t2_[:, f, :]) for f in range(NK)]
                        pairs.append((ones_b[0:1, 0:128], t2_[0:1, NK, :]))
                        k.op("pe", mm(b, psum[:, b, 0:WC], pairs), reads=[bf2, c2] + actb, writes=[pbuf[b]])
                        k.op("act", lambda e: e.copy(Ye[:, jt, db * WC:(db + 1) * WC], psum[:, b, 0:WC]), reads=[pbuf[b]], writes=[Yeb])
                for i in range(9):
                    for cg in range(4):
                        b = bank()
                        pairs = [(St_[:, jt, i * 128:(i + 1) * 128], Ye[:, jt, cg * 512:(cg + 1) * 512]) for jt in range(NJT)]
                        k.op("pe", mm(b, psum[:, b, 0:512], pairs), reads=[STb[r_], Yeb], writes=[pbuf[b]])
                        av = acc[:, i, cg * 512:(cg + 1) * 512]
                        k.op("dve", lambda e: e.scalar_tensor_tensor(av, psum[:, b, 0:512], comb[:, i, ex_:ex_ + 1], av, ALU.mult, ALU.add),
                             reads=[pbuf[b], combb, accb[i]], writes=[accb[i]])
            for ex_ in range(n_exp):
                emit_expert(ex_, 0)
                for w in range(1, NWIN):
                    k.cond_region(flags_d[0:1, w * NE + ex_:w * NE + ex_ + 1], lambda: emit_expert(ex_, w))
            k.barrier()

        with ExitStack() as s1:
            gf = sb([128, D], F32, "gf", s1)
            gfb = k.buf("gf")
            k.dma("sp", gf[:], normf_bc, gfb, writes=[gfb])
            m5t = [sb([128, D], F32, f"m5t{i}", s1) for i in range(2)]
            m5tb = k.buf("m5t")
            k.dma("sp", m5t[0][:], m5_d[0], m5tb, reads=[x1b[0]], writes=[m5tb])
            k.dma("sp", m5t[1][:], m5_d[1], m5tb, reads=[x1b[0]], writes=[m5tb], join=True)
            xf = [sb([128, D], F32, f"xf{i}", s1) for i in range(2)]
            xfb = [k.buf(f"xf{i}") for i in range(2)]
            junk2 = sb([128, D], BF16, "junk2", s1)
            j2b = k.buf("junk2")
            st2 = sb([128, 8], F32, "st2", s1)
            st2b = k.buf("st2")
            for i in range(9):
                xt, xb = xf[i % 2], xfb[i % 2]
                k.dma("sp", xt[:], x1_d[i * 128:(i + 1) * 128, :], xb, reads=[x1b[i]], writes=[xb])
                mt = m5t[1 if i == 8 else 0]
                k.op("dve", lambda e: e.tensor_tensor(acc[:, i, :], acc[:, i, :], mt[:], ALU.mult), reads=[accb[i], m5tb], writes=[accb[i]])
                k.op("dve", lambda e: e.tensor_tensor(xt[:], xt[:], acc[:, i, :], ALU.add), reads=[xb, accb[i]], writes=[xb])
                k.op("act", lambda e: e.activation(junk2[:], xt[:], AF.Square, accum_out=st2[:, 0:1]), reads=[xb], writes=[j2b, st2b])
                k.op("dve", lambda e: e.tensor_scalar(st2[:, 1:2], st2[:, 0:1], 1.0 / D, EPS, ALU.mult, ALU.add), reads=[st2b], writes=[st2b])
                k.op("act", lambda e: e.activation(st2[:, 3:4], st2[:, 1:2], AF.Sqrt), reads=[st2b], writes=[st2b])
                k.op("dve", lambda e: e.reciprocal(st2[:, 2:3], st2[:, 3:4]), reads=[st2b], writes=[st2b])
                k.op("dve", lambda e: e.scalar_tensor_tensor(xt[:], xt[:], st2[:, 2:3], gf[:], ALU.mult, ALU.mult), reads=[xb, st2b, gfb], writes=[xb])
                k.dma("sp", y_out[i * 128:(i + 1) * 128, :], xt[:], xb, reads=[xb], store=True)
            k.barrier()
        k.finish()
    return nc

STAGE = 9
NEG = -30000.0


def _core_inputs(c, I, stage, n_exp=NE):
    pb, half = c // 2, c % 2
    f = np.float32
    xp = I["x_prompt"][pb]
    own = xp[half * TP:(half + 1) * TP]
    halo = xp[half * TP - TH:half * TP] if half == 1 else np.zeros((TH, D), f)
    xs = I["x_sample"][NB * c:NB * (c + 1)].reshape(TS, D)
    i = np.arange(128)[:, None]
    j = np.arange(256)[None, :]
    diff = i + 128 - j
    band = (diff >= 0) & (diff < 128)
    maskp = np.where(band, 0.0, NEG).astype(f)
    maskp0 = maskp if half == 1 else np.where(band & (j >= 128), 0.0, NEG).astype(f)
    l = (np.arange(32) % 8)[:, None]
    maskc = np.where(np.arange(128)[None, :] > l, 0.0, NEG).astype(f)
    xw = np.arange(248)[None, :] - 120
    maskw = np.where((xw >= 0) & (xw <= l), 0.0, NEG).astype(f)
    sel = np.zeros((NR, 256), f)
    sel[0, 0:128] = 1.0
    for b in range(NB):
        sel[1 + b, 128 + 8 * b:128 + 8 * b + 8] = 1.0
    sk = I["sinks"]
    sinks_s = np.zeros((32, 8), f)
    for r in range(32):
        for g in range(4):
            for eo in range(2):
                sinks_s[r, g * 2 + eo] = sk[g * 8 + (r // 8) * 2 + eo]
    m = {
        "xin": np.concatenate([halo, own, xs], 0),
        "c_own": np.concatenate([I["c_prompt"][pb:pb + 1], I["c_sample"][NB * c:NB * (c + 1)], np.zeros((NR - 17, D), f)], 0),
        "cache_k": I["cache_k"][NB * c:NB * (c + 1)].reshape(NB, 128, 256),
        "cache_v": I["cache_v"][NB * c:NB * (c + 1)].reshape(NB, 128, 256),
        "state_conv": I["state_conv"][NB * c:NB * (c + 1)],
        "w_ada": I["w_ada"], "b_ada": I["b_ada"].reshape(1, -1),
        "w_in": I["w_in"], "b_in": I["b_in"].reshape(1, -1),
        "vec5": np.concatenate([I["norm1_g"], I["conv_dw_b"], I["conv_ln_g"], I["conv_ln_b"], I["norm2_g"]]).reshape(80, 128),
        "conv_dw_w": I["conv_dw_w"],
        "w_conv_out": I["w_conv_out"], "b_conv_out": I["b_conv_out"].reshape(1, -1),
        "sinks_bc": np.broadcast_to(sk[None, :], (128, 32)),
        "w_attn_out": I["w_attn_out"], "b_attn_out": I["b_attn_out"].reshape(1, -1),
        "w_out": I["w_out"], "w_router": I["w_router"], "b_router": I["b_router"].reshape(1, -1),
        "normf_bc": np.broadcast_to(I["norm_f_g"][None, :], (128, D)),
        "ident": np.eye(128, dtype=f), "maskp0": maskp0, "maskp": maskp, "maskc": maskc, "maskw": maskw,
        "hv": np.full((128, 1), float(half), f), "sinks_s": sinks_s, "sel": sel,
        "utri": (np.arange(128)[:, None] < np.arange(128)[None, :]).astype(f),
        "iotac": np.broadcast_to(np.arange(1152, dtype=f)[None, :], (128, 1152)),
        "iotap": np.stack([np.arange(128, dtype=f) + 128 * q for q in range(9)], 1),
    }
    if stage >= 5:
        m["w_mlp1"] = I["w_mlp1"][:n_exp]
        m["b_mlp1"] = I["b_mlp1"][:n_exp].reshape(n_exp, 1, 2 * D)
        m["w_mlp2"] = I["w_mlp2"][:n_exp]
        m["b_mlp2"] = I["b_mlp2"][:n_exp].reshape(n_exp, 1, D)
    return {k_: np.ascontiguousarray(v, dtype=f) for k_, v in m.items()}


def _run(I, stage, cores, dbg_names=(), n_exp=NE):
    nc = build(stage=stage, n_exp=n_exp, dbg_names=dbg_names)
    in_maps = [_core_inputs(c, I, stage, n_exp) for c in cores]
    res = run_bass_kernel_spmd(nc, in_maps, core_ids=list(range(len(cores))))
    return res.results


def kernel(**I):
    I = {k_: np.asarray(v) for k_, v in I.items()}
    R = _run(I, STAGE, list(range(8)))
    f = np.float32
    y_p = np.zeros((4, 2048, D), f)
    y_s = np.zeros((128, 8, D), f)
    k_p = np.zeros((4, 128, 4, 64), f)
    v_p = np.zeros((4, 128, 4, 64), f)
    cv_p = np.zeros((4, 30, D), f)
    k_s = np.zeros((128, 128, 4, 64), f)
    v_s = np.zeros((128, 128, 4, 64), f)
    cv_s = np.zeros((128, 30, D), f)
    for c in range(8):
        r = R[c]
        pb, half = c // 2, c % 2
        y_p[pb, half * TP:(half + 1) * TP] = r["y"][0:TP]
        y_s[NB * c:NB * (c + 1)] = r["y"][TP:].reshape(NB, 8, D)
        if half == 1:
            k_p[pb] = r["klast"].reshape(128, 4, 64)
            v_p[pb] = r["vlast"].reshape(128, 4, 64)
            cv_p[pb] = r["convlast"]
        k_s[NB * c:NB * (c + 1)] = r["ks_out"].reshape(NB, 128, 4, 64)
        v_s[NB * c:NB * (c + 1)] = r["vs_out"].reshape(NB, 128, 4, 64)
        cv_s[NB * c:NB * (c + 1)] = r["convs_out"]
    return (y_p, y_s, k_p, v_p, cv_p, k_s, v_s, cv_s)
```
